# Optimizing a Trainium2 kernel written in Bass

```python
import math
import jax, jax.numpy as jnp
from jax import lax
import numpy as np

D_MODEL = 1024
BATCH = 8
SEQ = 2048
DEPTH = 2

PLE_DIM = 256
BLOCK_Q = 128
DIFF_HEADS = 4
DIFF_DK = 64
DIFF_DV = 2 * DIFF_DK
RET_HEADS = 4
RET_DK = 64
RET_DV = 128
RET_CHUNK = 128
FOX_HEADS = 16
FOX_DH = D_MODEL // FOX_HEADS
REL_BUCKETS = 32
REL_MAX_DIST = 128
N_GROUPS = 4
EXPERTS_PER_GROUP = 4
N_EXPERTS = N_GROUPS * EXPERTS_PER_GROUP
TOP_K = 2
D_FF_EXPERT = D_MODEL // 2
DEEPNORM_ALPHA = (2 * DEPTH) ** 0.25
DEEPNORM_BETA = (8 * DEPTH) ** -0.25
LN_EPS = 1e-5
ROPE_BASE = 10000.0

A_QK = DIFF_HEADS * 2 * DIFF_DK
A_V = DIFF_HEADS * DIFF_DV
B_QK = RET_HEADS * RET_DK
B_V = RET_HEADS * RET_DV
EVEN_IN = 2 * A_QK + A_V + 2 * B_QK + 2 * B_V
EVEN_OUT = A_V + B_V
ODD_IN = 3 * FOX_HEADS * FOX_DH + FOX_HEADS

kernel_name = "hybrid_diffattn_retnet_fox_groupmoe"


def layer_norm(x, g, b):
    xf = x.astype(jnp.float32)
    mu = jnp.mean(xf, -1, keepdims=True)
    var = jnp.mean(jnp.square(xf - mu), -1, keepdims=True)
    return ((xf - mu) * lax.rsqrt(var + LN_EPS) * g.astype(jnp.float32) + b.astype(jnp.float32)).astype(x.dtype)


def head_rms_norm(y, g):
    yf = y.astype(jnp.float32)
    yf = yf * lax.rsqrt(jnp.mean(jnp.square(yf), -1, keepdims=True) + LN_EPS)
    return (yf * g.astype(jnp.float32)).astype(y.dtype)


def head_layer_norm(y, g):
    yf = y.astype(jnp.float32)
    mu = jnp.mean(yf, -1, keepdims=True)
    var = jnp.mean(jnp.square(yf - mu), -1, keepdims=True)
    return ((yf - mu) * lax.rsqrt(var + LN_EPS) * g.astype(jnp.float32)).astype(y.dtype)


def to_heads(t, n_heads):
    b, s, _ = t.shape
    return t.reshape(b, s, n_heads, -1).transpose(0, 2, 1, 3)


def merge_heads(t):
    b, h, s, d = t.shape
    return t.transpose(0, 2, 1, 3).reshape(b, s, h * d)


def rotary(x, pos):
    half = x.shape[-1] // 2
    inv = ROPE_BASE ** (-jnp.arange(half, dtype=jnp.float32) / half)
    ang = pos.astype(jnp.float32)[:, None] * inv[None, :]
    cos, sin = jnp.cos(ang), jnp.sin(ang)
    x1, x2 = x[..., :half], x[..., half:]
    return jnp.concatenate([x1 * cos - x2 * sin, x2 * cos + x1 * sin], -1).astype(x.dtype)


def t5_bucket(dist):
    max_exact = REL_BUCKETS // 2
    d = jnp.maximum(dist, 1).astype(jnp.float32)
    large = max_exact + (jnp.log(d / max_exact) / math.log(REL_MAX_DIST / max_exact)
                         * (REL_BUCKETS - max_exact)).astype(jnp.int32)
    large = jnp.minimum(large, REL_BUCKETS - 1)
    return jnp.where(dist < max_exact, dist, large)


def sweep_query_blocks(fn, seq):
    out = lax.map(fn, jnp.arange(seq // BLOCK_Q) * BLOCK_Q)
    n, b, h, q, d = out.shape
    return out.transpose(1, 2, 0, 3, 4).reshape(b, h, n * q, d)


def diff_attention(q1, q2, k1, k2, v, rel_bias, lam):
    seq = q1.shape[2]
    key_pos = jnp.arange(seq)
    scale = DIFF_DK ** -0.5

    def block(start):
        q_pos = start + jnp.arange(BLOCK_Q)
        dist = q_pos[:, None] - key_pos[None, :]
        causal = dist >= 0
        bias = jnp.moveaxis(rel_bias[t5_bucket(jnp.maximum(dist, 0))], -1, 0).astype(jnp.float32)

        def probs(q, k):
            qb = lax.dynamic_slice_in_dim(q, start, BLOCK_Q, axis=2)
            s = jnp.einsum('bhqd,bhkd->bhqk', qb, k).astype(jnp.float32) * scale + bias
            return jax.nn.softmax(jnp.where(causal, s, -jnp.inf), axis=-1)

        a = probs(q1, k1) - lam * probs(q2, k2)
        return jnp.einsum('bhqk,bhkd->bhqd', a.astype(v.dtype), v)

    return sweep_query_blocks(block, seq)


def retention(q, k, v):
    b, h, s, dk = q.shape
    dv = v.shape[-1]
    c = RET_CHUNK
    n = s // c
    log_g = jnp.log1p(-jnp.exp2(-5.0 - jnp.arange(h, dtype=jnp.float32)))
    j = jnp.arange(c, dtype=jnp.float32)
    rel = j[:, None] - j[None, :]
    decay_in = jnp.where(rel >= 0, jnp.exp(jnp.maximum(rel, 0.0)[None] * log_g[:, None, None]), 0.0)
    q_decay = jnp.exp((j + 1.0)[None] * log_g[:, None])
    k_decay = jnp.exp((c - 1.0 - j)[None] * log_g[:, None])
    chunk_decay = jnp.exp(c * log_g)

    qc = q.reshape(b, h, n, c, dk)
    kc = k.reshape(b, h, n, c, dk)
    vc = v.reshape(b, h, n, c, dv)
    scores = jnp.einsum('bhncd,bhnmd->bhncm', qc, kc) * decay_in[None, :, None]
    inner = jnp.einsum('bhncm,bhnme->bhnce', scores, vc)
    kv = jnp.einsum('bhnmd,bhnme->bhnde', kc * k_decay[None, :, None, :, None], vc)

    def step(state, kv_i):
        return chunk_decay[None, :, None, None] * state + kv_i, state

    init = jnp.zeros((b, h, dk, dv), kv.dtype)
    _, state_prev = lax.scan(step, init, jnp.moveaxis(kv, 2, 0))
    state_prev = jnp.moveaxis(state_prev, 0, 2)
    cross = jnp.einsum('bhncd,bhnde->bhnce', qc * q_decay[None, :, None, :, None], state_prev)
    return (inner + cross).reshape(b, h, s, dv).astype(v.dtype)


def forgetting_attention(q, k, v, log_f):
    seq = q.shape[2]
    key_pos = jnp.arange(seq)
    scale = FOX_DH ** -0.5
    cum = jnp.cumsum(log_f.astype(jnp.float32), axis=-1)

    def block(start):
        q_pos = start + jnp.arange(BLOCK_Q)
        causal = q_pos[:, None] >= key_pos[None, :]
        qb = lax.dynamic_slice_in_dim(q, start, BLOCK_Q, axis=2)
        cq = lax.dynamic_slice_in_dim(cum, start, BLOCK_Q, axis=2)
        s = (jnp.einsum('bhqd,bhkd->bhqk', qb, k).astype(jnp.float32) * scale
             + cq[..., :, None] - cum[..., None, :])
        pr = jax.nn.softmax(jnp.where(causal, s, -jnp.inf), axis=-1)
        return jnp.einsum('bhqk,bhkd->bhqd', pr.astype(v.dtype), v)

    return sweep_query_blocks(block, seq)


def even_mixer(x, w_in, w_out, lam_params, diff_g, ret_g, rel_bias, layer_idx):
    b, s, _ = x.shape
    u = x @ w_in
    cuts = np.cumsum([A_QK, A_QK, A_V, B_QK, B_QK, B_V])
    qa, ka, va, qb, kb, vb, gb = jnp.split(u, cuts, axis=-1)
    qa = qa.reshape(b, s, DIFF_HEADS, 2, DIFF_DK).transpose(0, 2, 3, 1, 4)
    ka = ka.reshape(b, s, DIFF_HEADS, 2, DIFF_DK).transpose(0, 2, 3, 1, 4)
    va = to_heads(va, DIFF_HEADS)
    lam_init = 0.8 - 0.6 * math.exp(-0.3 * layer_idx)
    lp = lam_params.astype(jnp.float32)
    lam = jnp.exp(jnp.sum(lp[0] * lp[1])) - jnp.exp(jnp.sum(lp[2] * lp[3])) + lam_init
    ya = diff_attention(qa[:, :, 0], qa[:, :, 1], ka[:, :, 0], ka[:, :, 1], va, rel_bias, lam)
    ya = head_rms_norm(ya, diff_g) * (1.0 - lam_init)
    pos = jnp.arange(s)
    qr = rotary(to_heads(qb, RET_HEADS), pos)
    kr = rotary(to_heads(kb, RET_HEADS), pos) * (RET_DK ** -0.5)
    yb = head_layer_norm(retention(qr, kr, to_heads(vb, RET_HEADS)), ret_g)
    yb = jax.nn.silu(gb) * merge_heads(yb)
    y = jnp.concatenate([merge_heads(ya).astype(x.dtype), yb.astype(x.dtype)], axis=-1)
    return y @ w_out


def odd_mixer(x, w_in, b_forget, w_out):
    u = x @ w_in
    width = FOX_HEADS * FOX_DH
    q, k, v, f_logit = jnp.split(u, [width, 2 * width, 3 * width], axis=-1)
    log_f = jax.nn.log_sigmoid((f_logit + b_forget).astype(jnp.float32)).transpose(0, 2, 1)
    y = forgetting_attention(to_heads(q, FOX_HEADS), to_heads(k, FOX_HEADS), to_heads(v, FOX_HEADS), log_f)
    return merge_heads(y) @ w_out


def grouped_moe(h, router_w, w_gate, w_up, w_down):
    b, s, _ = h.shape
    probs = jax.nn.softmax(jnp.einsum('bsd,de->bse', h, router_w).astype(jnp.float32), axis=-1)
    grp = probs.reshape(b, s, N_GROUPS, EXPERTS_PER_GROUP)
    grp_score = jnp.sum(lax.top_k(grp, TOP_K)[0], axis=-1)
    best = jnp.argmax(grp_score, axis=-1)
    in_group = jnp.repeat(jax.nn.one_hot(best, N_GROUPS, dtype=jnp.bool_), EXPERTS_PER_GROUP, axis=-1)
    vals, idx = lax.top_k(jnp.where(in_group, probs, -1.0), TOP_K)
    gates = vals / jnp.sum(vals, -1, keepdims=True)
    dense_gate = jnp.sum(jax.nn.one_hot(idx, N_EXPERTS, dtype=jnp.float32) * gates[..., None], axis=-2)
    dense_gate = dense_gate.astype(h.dtype)
    y = jnp.zeros_like(h)
    for e in range(N_EXPERTS):
        a = jax.nn.silu(h @ w_gate[e]) * (h @ w_up[e])
        y = y + dense_gate[..., e:e + 1] * (a @ w_down[e])
    return y


def setup_inputs(seed: int = 0) -> dict:
    key = jax.random.key(seed)
    ks = jax.random.split(key, 24)
    f32 = jnp.float32
    n_even = (DEPTH + 1) // 2
    n_odd = DEPTH // 2
    beta = DEEPNORM_BETA

    def nrm(k, shape, scale):
        return jax.random.normal(k, shape, f32) * scale

    even_cols = np.concatenate([np.ones(2 * A_QK), np.full(A_V, beta), np.ones(2 * B_QK),
                                np.full(B_V, beta), np.ones(B_V)]).astype(np.float32)
    odd_cols = np.concatenate([np.ones(2 * FOX_HEADS * FOX_DH), np.full(FOX_HEADS * FOX_DH, beta),
                               np.full(FOX_HEADS, 0.5)]).astype(np.float32)
    return {
        "x": nrm(ks[0], (BATCH, SEQ, D_MODEL), 1.0),
        "p": nrm(ks[1], (DEPTH, BATCH, SEQ, PLE_DIM), 1.0),
        "rel_bias": nrm(ks[2], (REL_BUCKETS, DIFF_HEADS), 0.5),
        "router_w": nrm(ks[3], (D_MODEL, N_EXPERTS), D_MODEL ** -0.5),
        "even_w_in": nrm(ks[4], (n_even, D_MODEL, EVEN_IN), D_MODEL ** -0.5) * jnp.asarray(even_cols),
        "even_w_out": nrm(ks[5], (n_even, EVEN_OUT, D_MODEL), beta * EVEN_OUT ** -0.5),
        "even_lambda": nrm(ks[6], (n_even, 4, DIFF_DK), 0.1),
        "even_diff_norm": 1.0 + nrm(ks[7], (n_even, DIFF_DV), 0.02),
        "even_ret_norm": 1.0 + nrm(ks[8], (n_even, RET_DV), 0.02),
        "odd_w_in": nrm(ks[9], (n_odd, D_MODEL, ODD_IN), D_MODEL ** -0.5) * jnp.asarray(odd_cols),
        "odd_b_forget": jax.random.uniform(ks[10], (n_odd, FOX_HEADS), f32, 1.0, 4.0),
        "odd_w_out": nrm(ks[11], (n_odd, FOX_HEADS * FOX_DH, D_MODEL), beta * D_MODEL ** -0.5),
        "ln_mix_g": 1.0 + nrm(ks[12], (DEPTH, D_MODEL), 0.02),
        "ln_mix_b": nrm(ks[13], (DEPTH, D_MODEL), 0.02),
        "ln_ffn_g": 1.0 + nrm(ks[14], (DEPTH, D_MODEL), 0.02),
        "ln_ffn_b": nrm(ks[15], (DEPTH, D_MODEL), 0.02),
        "moe_w_gate": nrm(ks[16], (DEPTH, N_EXPERTS, D_MODEL, D_FF_EXPERT), beta * D_MODEL ** -0.5),
        "moe_w_up": nrm(ks[17], (DEPTH, N_EXPERTS, D_MODEL, D_FF_EXPERT), beta * D_MODEL ** -0.5),
        "moe_w_down": nrm(ks[18], (DEPTH, N_EXPERTS, D_FF_EXPERT, D_MODEL), beta * D_FF_EXPERT ** -0.5),
        "ple_proj": nrm(ks[19], (DEPTH, PLE_DIM, D_MODEL), 0.5 * PLE_DIM ** -0.5),
        "ple_gate": nrm(ks[20], (DEPTH, D_MODEL, D_MODEL), D_MODEL ** -0.5),
    }


def reference(x, p, rel_bias, router_w, even_w_in, even_w_out, even_lambda, even_diff_norm,
              even_ret_norm, odd_w_in, odd_b_forget, odd_w_out, ln_mix_g, ln_mix_b,
              ln_ffn_g, ln_ffn_b, moe_w_gate, moe_w_up, moe_w_down, ple_proj, ple_gate):
    for i in range(DEPTH):
        j = i // 2
        if i % 2 == 0:
            mix = even_mixer(x, even_w_in[j], even_w_out[j], even_lambda[j], even_diff_norm[j],
                             even_ret_norm[j], rel_bias, i)
        else:
            mix = odd_mixer(x, odd_w_in[j], odd_b_forget[j], odd_w_out[j])
        h = layer_norm(DEEPNORM_ALPHA * x + mix.astype(x.dtype), ln_mix_g[i], ln_mix_b[i])
        ffn = grouped_moe(h, router_w, moe_w_gate[i], moe_w_up[i], moe_w_down[i])
        h = layer_norm(DEEPNORM_ALPHA * h + ffn, ln_ffn_g[i], ln_ffn_b[i])
        x = h + jax.nn.sigmoid(h @ ple_gate[i]) * (p[i] @ ple_proj[i])
    return x
```

```python
import math
from contextlib import ExitStack

import numpy as np
import concourse.bass as bass
import concourse.mybir as mybir
from concourse.bass_utils import run_bass_kernel_spmd

F32 = mybir.dt.float32
BF16 = mybir.dt.bfloat16
AF = mybir.ActivationFunctionType
ALU = mybir.AluOpType
AX = mybir.AxisListType

S = 2048
D = 1024
NT = 16
ALPHA = float((2 * 2) ** 0.25)
EPS = 1e-5
DMA_RING = 8
COMPUTE = ("pe", "act", "dve", "pool")


class T:
    __slots__ = ("name", "last_w", "readers")

    def __init__(self, name=""):
        self.name = name
        self.last_w = None
        self.readers = []


class Instr:
    __slots__ = ("idx", "eng", "fn", "deps", "is_dma", "signal", "count", "ring", "ringval", "kq")

    def __init__(self, idx, eng, fn, is_dma):
        self.idx = idx
        self.eng = eng
        self.fn = fn
        self.deps = set()
        self.is_dma = is_dma
        self.signal = is_dma
        self.count = None
        self.ring = None
        self.ringval = None
        self.kq = None


class Prog:
    def __init__(self, nc):
        self.nc = nc
        self.instrs = []
        self.streams = {e: [] for e in ("pe", "act", "dve", "pool", "sp")}
        self.dma_count = {e: 0 for e in ("sp", "act", "pool")}
        self.pending = {}
        self.dmas_since_barrier = set()
        self.cap = None

    def begin_capture(self):
        self.cap = []

    def end_capture(self):
        lst, self.cap = self.cap, None
        return lst

    def replay(self, lists):
        idx = [0] * len(lists)
        left = sum(len(l) for l in lists)
        while left:
            for k, l in enumerate(lists):
                if idx[k] < len(l):
                    kind, a, b, c, r, w = l[idx[k]]
                    idx[k] += 1
                    left -= 1
                    if kind == "op":
                        self.op(a, b, r, w)
                    else:
                        self.dma(a, b, c, r, w)

    def barrier(self):
        deps = set(self.dmas_since_barrier)
        self.dmas_since_barrier = set()
        for e, st in self.streams.items():
            for ins in reversed(st):
                if not ins.is_dma:
                    deps.add(ins.idx)
                    break
        for e in self.streams:
            self.pending[e] = self.pending.get(e, set()) | deps

    def _add(self, eng, fn, reads, writes, is_dma):
        ins = Instr(len(self.instrs), eng, fn, is_dma)
        self.instrs.append(ins)
        self.streams[eng].append(ins)
        if eng in self.pending:
            ins.deps |= self.pending.pop(eng)
        for t in reads:
            if t.last_w is not None:
                ins.deps.add(t.last_w)
        for t in writes:
            if t.last_w is not None:
                ins.deps.add(t.last_w)
            for r in t.readers:
                ins.deps.add(r)
        for t in writes:
            t.last_w = ins.idx
            t.readers = []
        for t in reads:
            if t.last_w == ins.idx:
                continue
            if not is_dma:
                t.readers = [r for r in t.readers
                             if self.instrs[r].is_dma or self.instrs[r].eng != eng]
            t.readers.append(ins.idx)
        ins.deps.discard(ins.idx)
        if is_dma:
            self.dmas_since_barrier.add(ins.idx)
        return ins

    def op(self, eng, fn, reads=(), writes=()):
        if self.cap is not None:
            self.cap.append(("op", eng, fn, None, list(reads), list(writes)))
            return None
        return self._add(eng, fn, reads, writes, False)

    def dma(self, q, out, in_, reads=(), writes=()):
        if self.cap is not None:
            self.cap.append(("dma", q, out, in_, list(reads), list(writes)))
            return None

        def fn(e, out=out, in_=in_):
            return e.dma_start(out=out, in_=in_)
        ins = self._add(q, fn, reads, writes, True)
        ins.kq = self.dma_count[q]
        self.dma_count[q] += 1
        return ins

    def emit(self):
        nc = self.nc
        instrs = self.instrs
        for ins in instrs:
            nd = set()
            for d in ins.deps:
                di = instrs[d]
                if di.eng == "pe" and ins.eng == "pe" and not di.is_dma and not ins.is_dma:
                    continue
                nd.add(d)
            ins.deps = nd
            for d in nd:
                instrs[d].signal = True
        for e, st in self.streams.items():
            c = 0
            for ins in st:
                if ins.is_dma:
                    continue
                if ins.signal:
                    c += 1
                    ins.count = c
        with ExitStack() as es:
            sem_eng = {e: es.enter_context(nc.semaphore("s_" + e)) for e in COMPUTE}
            rings = {}
            for q in ("sp", "act", "pool"):
                if self.dma_count[q]:
                    rings[q] = [es.enter_context(nc.semaphore("r_%s%d" % (q, i)))
                                for i in range(DMA_RING)]
            for ins in instrs:
                if ins.is_dma:
                    ins.ring = rings[ins.eng][ins.kq % DMA_RING]
                    ins.ringval = 16 * (ins.kq // DMA_RING + 1)
            block = es.enter_context(nc.Block())

            def run_stream(ename, eobj):
                known = {}
                last_dma = {}
                for ins in self.streams[ename]:
                    waits = {}
                    for d in ins.deps:
                        di = instrs[d]
                        if di.is_dma:
                            sem, val = di.ring, di.ringval
                        else:
                            sem, val = sem_eng[di.eng], di.count
                        key = id(sem)
                        if known.get(key, 0) >= val:
                            continue
                        if key not in waits or waits[key][1] < val:
                            waits[key] = (sem, val)
                    if ins.is_dma and ins.kq >= DMA_RING:
                        sem = ins.ring
                        val = ins.ringval - 16
                        key = id(sem)
                        if known.get(key, 0) < val and (key not in waits or waits[key][1] < val):
                            waits[key] = (sem, val)
                    for key, (sem, val) in waits.items():
                        eobj.wait_ge(sem, val)
                        known[key] = val
                    bi = ins.fn(eobj)
                    if ins.is_dma:
                        bi.then_inc(ins.ring, 16)
                        last_dma[id(ins.ring)] = (ins.ring, ins.ringval)
                    elif ins.signal:
                        bi.then_inc(sem_eng[ename], 1)
                for key, (sem, val) in last_dma.items():
                    if known.get(key, 0) < val:
                        eobj.wait_ge(sem, val)

            if self.streams["sp"]:
                @block.sync
                def _(e):
                    run_stream("sp", e)
            if self.streams["pe"]:
                @block.tensor
                def _(e):
                    run_stream("pe", e)
            if self.streams["act"]:
                @block.scalar
                def _(e):
                    run_stream("act", e)
            if self.streams["dve"]:
                @block.vector
                def _(e):
                    run_stream("dve", e)
            if self.streams["pool"]:
                @block.gpsimd
                def _(e):
                    run_stream("pool", e)


def _t5_bucket_np(dist):
    d = np.maximum(dist, 1).astype(np.float32)
    large = 16 + (np.log(d / np.float32(16)) / np.float32(math.log(128 / 16)) * np.float32(16)).astype(np.int32)
    large = np.minimum(large, 31)
    return np.where(dist < 16, dist, large)


C_COS = 0
C_SIN = 512
C_DM = 1024
C_QD = 1536
C_KD = 1792
NCONST = 1800


def _static_consts():
    c = np.zeros((128, NCONST), np.float32)
    p = np.arange(128)
    half = 32
    inv = (10000.0 ** (-np.arange(half, dtype=np.float32) / half)).astype(np.float32)
    for t in range(NT):
        pos = (t * 128 + p).astype(np.float32)
        ang = pos[:, None] * inv[None, :]
        c[:, C_COS + t * 32:C_COS + (t + 1) * 32] = np.cos(ang)
        c[:, C_SIN + t * 32:C_SIN + (t + 1) * 32] = np.sin(ang)
    log_g = np.log1p(-np.exp2(-5.0 - np.arange(4, dtype=np.float64)))
    cc = np.arange(128)
    for h in range(4):
        dm = np.where(cc[None, :] >= p[:, None], np.exp(-(p[:, None] + 1.0) * log_g[h]), 0.0)
        c[:, C_DM + h * 128:C_DM + (h + 1) * 128] = dm
        c[:, C_KD + h] = np.exp((127.0 - p) * log_g[h]) * 0.125
    for h in range(4):
        c[:, C_QD + h] = np.exp((p + 1.0) * log_g[h])
    cd = [float(np.exp(128.0 * log_g[h])) for h in range(4)]
    return c, cd


import os
STOP = os.environ.get('KSTOP', '')
RET_N = int(os.environ.get('KRETN', '16'))
STAGE = int(os.environ.get('KSTAGE', '9'))


def build(layers, debug=None):
    nc = bass.Bass("TRN2", target_bir_lowering=False)
    _, CD = _static_consts()

    def din(name, shape):
        return nc.dram_tensor(name, list(shape), F32, kind="ExternalInput").ap()

    x_d = din("x", [S, D])
    consts_d = din("consts", [128, NCONST])
    router_d = din("router_w", [D, 16])
    Ld = {}
    for l in layers:
        d = {}
        d["p"] = din("p%d" % l, [S, 256])
        d["w_in"] = din("w_in%d" % l, [D, 3072 if l == 0 else 3088])
        d["w_out"] = din("w_out%d" % l, [D, D])
        d["lnp"] = din("lnp%d" % l, [4, D])
        d["wg"] = din("wg%d" % l, [16, D, 512])
        d["wu"] = din("wu%d" % l, [16, D, 512])
        d["wd"] = din("wd%d" % l, [16, 512, D])
        d["pproj"] = din("pproj%d" % l, [256, D])
        d["pgate"] = din("pgate%d" % l, [D, D])
        if l == 0:
            d["relbT"] = din("relbT", [128, 4 * 256])
            d["lam"] = din("lam", [1, 256])
            d["dg"] = din("dg", [1, 128])
            d["rg"] = din("rg", [1, 128])
        else:
            d["bf"] = din("bf", [1, 16])
        Ld[l] = d
    y_d = nc.dram_tensor("y", [S, D], F32, kind="ExternalOutput").ap()

    es = ExitStack()
    with es:
        def sb(n, s, d):
            return es.enter_context(nc.sbuf_tensor(n, list(s), d))

        X = sb("X", [128, NT, D], F32)
        XT = sb("XT", [128, 8, S], BF16)
        YTb = sb("YTb", [128, 4, S], BF16)
        LNP = sb("LNP", [128, 2048], F32)
        BIG = sb("BIG", [128, 37184], BF16)
        ident = sb("ident", [128, 128], BF16)
        trim = sb("trim", [128, 128], BF16)
        HB = sb("HB", [128, 2, D], BF16)
        PT = sb("PT", [128, 4, 512], BF16)
        BT = sb("BT", [128, 4, 256], F32)
        SM = sb("SM", [128, 512], F32)
        EPSB = sb("EPSB", [128, 1], F32)
        PS = [es.enter_context(nc.psum_tensor("ps%d" % i, [128, 512], F32)) for i in range(8)]

        P = Prog(nc)
        tX = [T("X%d" % t) for t in range(NT)]
        tXT = [T("XT%d" % t) for t in range(NT)]
        tYTb = [T("YTb%d" % t) for t in range(NT)]
        tPS = [T("ps%d" % i) for i in range(8)]
        tW = [T("w%d" % i) for i in range(6)]
        tHB = [T("hb0"), T("hb1")]
        tPT = [T("pt%d" % i) for i in range(4)]
        tBT = [T("bt%d" % i) for i in range(4)]
        tid = T("ident")
        ttr = T("trim")
        tLNP = T("lnp")

        A0 = 3 * 4096
        E0 = 6 * 4096

        class Region:
            def __init__(self, base, size):
                self.base, self.size, self.cur = base, size, 0

            def reset(self):
                self.cur = 0

            def take(self, nbytes, dt=BF16):
                n = (nbytes + 3) // 4 * 2
                assert self.cur + n <= self.size, (self.cur, n, self.size)
                ap = BIG[:, self.base + self.cur:self.base + self.cur + n]
                self.cur += n
                if dt == F32:
                    ap = ap.bitcast(F32)
                return ap

        ARENA = Region(A0, 37184 - A0)
        EXTRA = Region(E0, 37184 - E0)

        def wslot(s, c, f=None):
            f = f or 4096 // c
            return BIG[:, s * 4096:s * 4096 + c * f].rearrange("p (c f) -> p c f", c=c)

        def load_w(s, dram_ap, c):
            P.dma("pool", wslot(s, c, dram_ap.shape[1]), dram_ap.rearrange("(c p) f -> p c f", p=128), writes=[tW[s]])

        P.op("dve", lambda e: e.memset(ident[:], 1.0), writes=[tid])
        P.op("dve", lambda e: e.memset(EPSB[:], EPS), writes=[tid])
        P.op("pool", lambda e: e.affine_select(out=ident[:], in_=ident[:], pattern=[[-1, 128]],
                                               compare_op=ALU.is_equal, fill=0.0, base=0, channel_multiplier=1),
             reads=[tid], writes=[tid])
        P.op("dve", lambda e: e.memset(trim[:], 1.0), writes=[ttr])
        P.op("pool", lambda e: e.affine_select(out=trim[:], in_=trim[:], pattern=[[1, 128]],
                                               compare_op=ALU.is_ge, fill=0.0, base=0, channel_multiplier=-1),
             reads=[ttr], writes=[ttr])

        cp_flip = [0]

        def evac(out, in_, reads, writes, scale=None):
            cp_flip[0] ^= 1
            if scale is not None:
                P.op("act", lambda e: e.mul(out=out, in_=in_, mul=scale), reads=reads, writes=writes)
            elif cp_flip[0]:
                P.op("act", lambda e: e.copy(out=out, in_=in_), reads=reads, writes=writes)
            else:
                P.op("dve", lambda e: e.tensor_copy(out=out, in_=in_), reads=reads, writes=writes)

        def transposes(src_fn, nblk, src_reads, bank, dst, dst_writes):
            pv = PS[bank][:].bitcast(BF16)
            for k in range(nblk):
                P.op("pe", lambda e, k=k: e.transpose(out=pv[:, k * 128:(k + 1) * 128], in_=src_fn(k), identity=ident[:]),
                     reads=list(src_reads) + [tid], writes=[tPS[bank]])
            evac(dst, pv[:, 0:nblk * 128].rearrange("p (c f) -> p c f", c=nblk), [tPS[bank]], dst_writes)

        def x_to_xt(t, bank, hb=None):
            if hb is None:
                hb = t % 2
            P.op("act", lambda e: e.copy(out=HB[:, hb, :], in_=X[:, t, :]), reads=[tX[t]], writes=[tHB[hb]])
            transposes(lambda k: HB[:, hb, k * 128:(k + 1) * 128], 8, [tHB[hb]], bank,
                       XT[:, :, t * 128:(t + 1) * 128], [tXT[t]])

        tSt = [T("st%d" % t) for t in range(NT)]

        def ln_group(tiles, gi):
            st = SM[:, 0:192].rearrange("p (t s) -> p t s", t=16)
            mv = SM[:, 192:256].rearrange("p (t s) -> p t s", t=16)
            t0, t1 = tiles[0], tiles[-1] + 1
            ts_ = [tSt[t] for t in tiles]
            for t in tiles:
                P.op("dve", lambda e, t=t: e.bn_stats(out=st[:, t, 0:6], in_=X[:, t, 0:512]), reads=[tX[t]], writes=[tSt[t]])
                P.op("dve", lambda e, t=t: e.bn_stats(out=st[:, t, 6:12], in_=X[:, t, 512:1024]), reads=[tX[t]], writes=[tSt[t]])
            for t in tiles:
                P.op("dve", lambda e, t=t: e.bn_aggr(out=mv[:, t, 0:2], in_=st[:, t, :]), reads=[tSt[t]], writes=[tSt[t]])
            P.op("act", lambda e: e.activation(out=mv[:, t0:t1, 2], in_=mv[:, t0:t1, 1], func=AF.Sqrt, bias=EPSB[:, 0:1]),
                 reads=ts_ + [tid], writes=ts_)
            P.op("dve", lambda e: e.reciprocal(out=mv[:, t0:t1, 2], in_=mv[:, t0:t1, 2]), reads=ts_, writes=ts_)
            P.op("dve", lambda e: e.scalar_tensor_tensor(out=mv[:, t0:t1, 3], in0=mv[:, t0:t1, 0], scalar=-1.0,
                                                         in1=mv[:, t0:t1, 2], op0=ALU.mult, op1=ALU.mult), reads=ts_, writes=ts_)
            for t in tiles:
                P.op("act", lambda e, t=t: e.activation(out=X[:, t, :], in_=X[:, t, :], func=AF.Identity,
                                                        scale=mv[:, t, 2:3], bias=mv[:, t, 3:4]), reads=[tX[t], tSt[t]], writes=[tX[t]])
            for t in tiles:
                P.op("dve", lambda e, t=t: e.tensor_tensor(out=X[:, t, :], in0=X[:, t, :], in1=LNP[:, gi * 1024:(gi + 1) * 1024],
                                                           op=ALU.mult), reads=[tX[t], tLNP], writes=[tX[t]])
                P.op("pool", lambda e, t=t: e.tensor_tensor(out=X[:, t, :], in0=X[:, t, :],
                                                            in1=LNP[:, (gi + 1) * 1024:(gi + 2) * 1024], op=ALU.add),
                     reads=[tX[t], tLNP], writes=[tX[t]])

        def load_lnp(l, which):
            P.dma("sp", LNP[:].rearrange("p (a f) -> p a f", a=2),
                  Ld[l]["lnp"][2 * which:2 * which + 2, :].unsqueeze(0).broadcast_to([128, 2, D]), writes=[tLNP])

        for t in range(NT):
            P.dma("sp", X[:, t, :], x_d[t * 128:(t + 1) * 128, :], writes=[tX[t]])
        xt_pending = []
        npre = 2 if layers[0] == 0 else NT
        for t in range(npre):
            x_to_xt(t, t % 2)
        xt_pending.extend(range(npre, NT))

        def proj_feat(ws, dst, dst_t, nchunk, banks):
            W = wslot(ws, 8)
            k = 0
            for ch in range(nchunk):
                for g in range(4):
                    b = banks[k % len(banks)]
                    k += 1
                    for c in range(8):
                        P.op("pe", lambda e, c=c, ch=ch, g=g, b=b: e.matmul(
                            PS[b][:], lhsT=W[:, c, ch * 128:(ch + 1) * 128], rhs=XT[:, c, g * 512:(g + 1) * 512],
                            start=(c == 0), stop=(c == 7)),
                            reads=[tW[ws]] + tXT[4 * g:4 * g + 4], writes=[tPS[b]])
                    evac(dst[:, ch, g * 512:(g + 1) * 512], PS[b][:], [tPS[b]], [dst_t[ch]])

        def proj_tok(ws, t, b, ncols=512):
            W = wslot(ws, 8)
            for c in range(8):
                P.op("pe", lambda e, c=c: e.matmul(PS[b][:, 0:ncols], lhsT=XT[:, c, t * 128:(t + 1) * 128],
                                                   rhs=W[:, c, 0:ncols], start=(c == 0), stop=(c == 7)),
                     reads=[tW[ws], tXT[t]], writes=[tPS[b]])

        PT8 = PT[:].rearrange("p a f -> p (a f)").rearrange("p (a f) -> p a f", a=8)
        tPT8 = [T("pt8_%d" % i) for i in range(8)]

        def attention(nunits, unit_part, unit_chunk, unit_v, QT, KT, tQ, tK, V, tV, dv1, G, bias_fn, near_fn,
                      epilogue, after_group, acc_map, diag_mask, look=1):
            ngroups = NT // G

            def unit_body(g, u, stream):
                nbuf = look + 1
                hp = unit_part(u)
                hc = unit_chunk(u)
                if look == 1:
                    sbk = [2 * stream, 2 * stream + 1]
                    S_ap = lambda j: PS[sbk[j % 2]][:]
                    S_t = lambda j: tPS[sbk[j % 2]]
                    PT_ap = lambda j: PT[:, 2 * stream + j % 2, :]
                    PT_t = lambda j: tPT[2 * stream + j % 2]
                else:
                    S_ap = lambda j: PS[3 * stream + j % 3][:]
                    S_t = lambda j: tPS[3 * stream + j % 3]
                    PT_ap = lambda j: PT8[:, 3 * stream + j % 3, :]
                    PT_t = lambda j: tPT8[3 * stream + j % 3]

                def acc(i, u=u):
                    b, off = acc_map(u, i)
                    return PS[b][:, off:off + dv1]

                def acc_t(i, u=u):
                    return tPS[acc_map(u, i)[0]]

                jmax = G * g + G - 1
                started = set()
                info = {}

                def score(j):
                    ilo = max(j, G * g)
                    nb = G * g + G - ilo
                    sap, st_, pap, pt_ = S_ap(j), S_t(j), PT_ap(j), PT_t(j)
                    P.op("pe", lambda e, j=j, ilo=ilo, nb=nb, sap=sap: e.matmul(
                        sap[:, 0:nb * 128], lhsT=KT[hp:hp + 64, hc, j * 128:(j + 1) * 128],
                        rhs=QT[hp:hp + 64, hc, ilo * 128:(ilo + nb) * 128], start=True, stop=True),
                        reads=[tK[hc], tQ[hc]], writes=[st_])
                    done = near_fn(u, g, j, ilo, nb, sap, st_, pap, pt_, stream)
                    if done < nb:
                        bias, breads = bias_fn(u, g, j)
                        P.op("act", lambda e, sap=sap, pap=pap, done=done, nb=nb, bias=bias: e.activation(
                            out=pap[:, done * 128:nb * 128], in_=sap[:, done * 128:nb * 128],
                            func=AF.Exp, scale=0.125, bias=bias),
                            reads=[st_] + breads, writes=[pt_])
                    if diag_mask and ilo == j:
                        P.op("dve", lambda e, pap=pap: e.tensor_tensor(out=pap[:, 0:128], in0=pap[:, 0:128],
                                                                       in1=trim[:], op=ALU.mult),
                             reads=[pt_, ttr], writes=[pt_])
                    info[j] = (j, ilo, nb, pap, pt_)

                for j in range(min(look, jmax + 1)):
                    score(j)
                for j in range(jmax + 1):
                    if j + look <= jmax:
                        score(j + look)
                    emit_pv(info.pop(j), u, g, acc, acc_t, V, tV, unit_v, dv1, G, started, acc_map)
                epilogue(u, g, acc, acc_t)

            for g in range(ngroups):
                for u0 in range(0, nunits, 2):
                    P.begin_capture()
                    unit_body(g, u0, 0)
                    la = P.end_capture()
                    P.begin_capture()
                    unit_body(g, u0 + 1, 1)
                    lb = P.end_capture()
                    P.replay([la, lb])
                after_group(g)

        def emit_pv(pend, u, g, acc, acc_t, V, tV, unit_v, dv1, G, started, acc_map):
            j, ilo, nb, pap, pt_ = pend
            for k in range(nb):
                i = ilo + k
                ii = i - G * g
                bank = acc_map(u, ii)[0]
                st = False
                if j == 0 and bank not in started:
                    started.add(bank)
                    st = True
                P.op("pe", lambda e, k=k, ii=ii, j=j, i=i, pap=pap, st=st: e.matmul(
                    acc(ii), lhsT=pap[:, k * 128:(k + 1) * 128], rhs=V[:, j, unit_v(u), 0:dv1],
                    start=st, stop=(j == i), skip_group_check=True),
                    reads=[pt_, tV[j]], writes=[acc_t(ii)])

        def ffn_block(l, last):
            d = Ld[l]
            if os.environ.get("KBAR", "0") == "1":
                P.barrier()
            EXTRA.reset()
            tE = T("extra")
            Wr32 = EXTRA.take(8 * 16 * 4, F32).rearrange("p (c f) -> p c f", c=8)
            Wr = EXTRA.take(8 * 16 * 2).rearrange("p (c f) -> p c f", c=8)
            tWr = T("wr")
            P.dma("sp", Wr32, router_d.rearrange("(c p) f -> p c f", p=128), writes=[tWr])
            P.op("dve", lambda e: e.tensor_copy(out=Wr, in_=Wr32), reads=[tWr], writes=[tWr])
            def load_expert(e_, base):
                load_w(base + 0, d["wg"][e_], 8)
                load_w(base + 1, d["wu"][e_], 8)
                load_w(base + 2, d["wd"][e_], 4)
            load_expert(0, 0)
            load_expert(1, 3)
            for t in range(NT):
                for c in range(8):
                    P.op("pe", lambda e, t=t, c=c: e.matmul(PS[7][:, t * 16:(t + 1) * 16], lhsT=XT[:, c, t * 128:(t + 1) * 128],
                                                         rhs=Wr[:, c, :], start=(c == 0), stop=(c == 7)),
                         reads=[tXT[t], tWr], writes=[tPS[7]])
            tG = T("gate")
            def f32t(n):
                return EXTRA.take(n * 4, F32)
            L = f32t(256); E_ = f32t(256); E2 = f32t(256); EQ = f32t(256); SEL = f32t(256); GATE = f32t(256)
            mx = f32t(16); m1 = f32t(64); m2 = f32t(64); gs = f32t(64); gm = f32t(16); gsel = f32t(64); den = f32t(16)
            L3 = L.rearrange("p (t e) -> p t e", t=16)
            def v4(ap):
                return ap.rearrange("p (t e) -> p t e", e=4)
            P.op("act", lambda e: e.copy(out=L, in_=PS[7][:, 0:256]), reads=[tPS[7]], writes=[tG])
            P.op("dve", lambda e: e.tensor_reduce(out=mx, in_=L3, axis=AX.X, op=ALU.max), reads=[tG], writes=[tG])
            P.op("dve", lambda e: e.tensor_tensor(out=L3, in0=L3, in1=mx.unsqueeze(2).broadcast_to([128, 16, 16]),
                                                  op=ALU.subtract), reads=[tG], writes=[tG])
            P.op("act", lambda e: e.activation(out=E_, in_=L, func=AF.Exp), reads=[tG], writes=[tG])
            P.op("dve", lambda e: e.tensor_reduce(out=m1, in_=v4(E_), axis=AX.X, op=ALU.max), reads=[tG], writes=[tG])
            P.op("dve", lambda e: e.tensor_tensor(out=v4(EQ), in0=v4(E_), in1=m1.unsqueeze(2).broadcast_to([128, 64, 4]),
                                                  op=ALU.is_equal), reads=[tG], writes=[tG])
            P.op("dve", lambda e: e.scalar_tensor_tensor(out=E2, in0=EQ, scalar=-4.0, in1=E_, op0=ALU.mult, op1=ALU.add),
                 reads=[tG], writes=[tG])
            P.op("dve", lambda e: e.tensor_reduce(out=m2, in_=v4(E2), axis=AX.X, op=ALU.max), reads=[tG], writes=[tG])
            P.op("dve", lambda e: e.tensor_tensor(out=gs, in0=m1, in1=m2, op=ALU.add), reads=[tG], writes=[tG])
            P.op("dve", lambda e: e.tensor_reduce(out=gm, in_=v4(gs), axis=AX.X, op=ALU.max), reads=[tG], writes=[tG])
            P.op("dve", lambda e: e.tensor_tensor(out=v4(gsel), in0=v4(gs), in1=gm.unsqueeze(2).broadcast_to([128, 16, 4]),
                                                  op=ALU.is_equal), reads=[tG], writes=[tG])
            P.op("dve", lambda e: e.tensor_tensor(out=v4(SEL), in0=v4(E_), in1=m2.unsqueeze(2).broadcast_to([128, 64, 4]),
                                                  op=ALU.is_ge), reads=[tG], writes=[tG])
            P.op("dve", lambda e: e.tensor_tensor(out=v4(SEL), in0=v4(SEL), in1=gsel.unsqueeze(2).broadcast_to([128, 64, 4]),
                                                  op=ALU.mult), reads=[tG], writes=[tG])
            P.op("dve", lambda e: e.tensor_tensor(out=E2, in0=E_, in1=SEL, op=ALU.mult), reads=[tG], writes=[tG])
            P.op("dve", lambda e: e.tensor_reduce(out=den, in_=E2.rearrange("p (t e) -> p t e", t=16), axis=AX.X, op=ALU.add),
                 reads=[tG], writes=[tG])
            P.op("dve", lambda e: e.reciprocal(out=den, in_=den), reads=[tG], writes=[tG])
            P.op("dve", lambda e: e.tensor_tensor(out=GATE.rearrange("p (t e) -> p t e", t=16),
                                                  in0=E2.rearrange("p (t e) -> p t e", t=16),
                                                  in1=den.unsqueeze(2).broadcast_to([128, 16, 16]), op=ALU.mult),
                 reads=[tG], writes=[tG])
            for t in range(NT):
                P.op("act", lambda e, t=t: e.mul(out=X[:, t, :], in_=X[:, t, :], mul=ALPHA), reads=[tX[t]], writes=[tX[t]])
            AT = [EXTRA.take(4 * 512 * 2).rearrange("p (c f) -> p c f", c=4) for _ in range(2)]
            tAT = [T("at0"), T("at1")]
            SG = [EXTRA.take(512 * 2) for _ in range(2)]
            tSG = [T("sg0"), T("sg1")]
            kc = {"gu": 0, "y": 0}

            def GU(e_, g):
                base = (e_ % 2) * 3
                Wg = wslot(base, 8); Wu = wslot(base + 1, 8)
                ab = g % 2
                for fc in range(4):
                    bg = (kc["gu"] % 2) * 2
                    bu = bg + 1
                    si = kc["gu"] % 2
                    kc["gu"] += 1
                    for c in range(8):
                        P.op("pe", lambda e, c=c, fc=fc, g=g, bg=bg, Wg=Wg: e.matmul(
                            PS[bg][:], lhsT=Wg[:, c, fc * 128:(fc + 1) * 128], rhs=XT[:, c, g * 512:(g + 1) * 512],
                            start=(c == 0), stop=(c == 7)), reads=[tW[base]] + tXT[4 * g:4 * g + 4], writes=[tPS[bg]])
                    for c in range(8):
                        P.op("pe", lambda e, c=c, fc=fc, g=g, bu=bu, Wu=Wu: e.matmul(
                            PS[bu][:], lhsT=Wu[:, c, fc * 128:(fc + 1) * 128], rhs=XT[:, c, g * 512:(g + 1) * 512],
                            start=(c == 0), stop=(c == 7)), reads=[tW[base + 1]] + tXT[4 * g:4 * g + 4], writes=[tPS[bu]])
                    P.op("act", lambda e, bg=bg, si=si: e.activation(out=SG[si], in_=PS[bg][:], func=AF.Silu),
                         reads=[tPS[bg]], writes=[tSG[si]])
                    P.op("dve", lambda e, bu=bu, si=si, ab=ab, fc=fc: e.tensor_tensor(
                        out=AT[ab][:, fc, :], in0=SG[si], in1=PS[bu][:], op=ALU.mult),
                        reads=[tSG[si], tPS[bu]], writes=[tAT[ab]])

            def DOWN(e_, g):
                base = (e_ % 2) * 3
                Wd = wslot(base + 2, 4)
                ab = g % 2
                for tt in range(4):
                    t = 4 * g + tt
                    for n in range(2):
                        by = 4 + (kc["y"] % 4)
                        kc["y"] += 1
                        for fc in range(4):
                            P.op("pe", lambda e, fc=fc, tt=tt, n=n, by=by, ab=ab, Wd=Wd: e.matmul(
                                PS[by][:], lhsT=AT[ab][:, fc, tt * 128:(tt + 1) * 128], rhs=Wd[:, fc, n * 512:(n + 1) * 512],
                                start=(fc == 0), stop=(fc == 3)), reads=[tAT[ab], tW[base + 2]], writes=[tPS[by]])
                        P.op("dve", lambda e, t=t, n=n, by=by, e_=e_: e.scalar_tensor_tensor(
                            out=X[:, t, n * 512:(n + 1) * 512], in0=PS[by][:], scalar=GATE[:, t * 16 + e_:t * 16 + e_ + 1],
                            in1=X[:, t, n * 512:(n + 1) * 512], op0=ALU.mult, op1=ALU.add),
                            reads=[tPS[by], tG, tX[t]], writes=[tX[t]])

            steps = [(e_, g) for e_ in range(16) for g in range(4)]
            GU(*steps[0])
            for i_, (e_, g) in enumerate(steps):
                if i_ + 1 < len(steps):
                    GU(*steps[i_ + 1])
                DOWN(e_, g)
                if g == 3 and e_ + 2 < 16:
                    load_expert(e_ + 2, (e_ % 2) * 3)
            load_lnp(l, 1)
            load_w(0, d["pgate"][:, 0:512], 8)
            load_w(1, d["pgate"][:, 512:1024], 8)
            load_w(2, d["pproj"], 2)
            Wp0 = wslot(0, 8); Wp1 = wslot(1, 8); Wpp = wslot(2, 2, 1024)
            P.barrier()
            EXTRA.reset()
            pT = EXTRA.take(2 * S * 2).rearrange("p (c f) -> p c f", c=2)
            tpT = [T("pT%d" % t) for t in range(NT)]
            P32 = [EXTRA.take(256 * 4, F32) for _ in range(4)]
            Pb = [EXTRA.take(256 * 2) for _ in range(4)]
            tP32 = [T("p32_%d" % i) for i in range(4)]
            tPb = [T("pb_%d" % i) for i in range(4)]
            SGF = [EXTRA.take(512 * 4, F32) for _ in range(4)]
            tSGF = [T("sgf%d" % i) for i in range(4)]

            def pprep():
                for t in range(NT):
                    k = t % 4
                    P.dma("sp", P32[k], d["p"][t * 128:(t + 1) * 128, :], writes=[tP32[k]])
                    P.op("act", lambda e, k=k: e.copy(out=Pb[k], in_=P32[k]), reads=[tP32[k]], writes=[tPb[k]])
                    transposes(lambda kk, k=k: Pb[k][:, kk * 128:(kk + 1) * 128], 2, [tPb[k]], 6,
                               pT[:, :, t * 128:(t + 1) * 128], [tpT[t]])
            kk = [0]

            def A(grp):
                ln_group(grp, 0)
                for t in grp:
                    x_to_xt(t, 7, 0)

            def B(grp):
                for t in grp:
                    for n in range(2):
                        ba = (kk[0] % 2) * 2
                        bb = ba + 1
                        si = kk[0] % 4
                        kk[0] += 1
                        Wp = Wp0 if n == 0 else Wp1
                        for c in range(8):
                            P.op("pe", lambda e, c=c, t=t, ba=ba, Wp=Wp: e.matmul(
                                PS[ba][:], lhsT=XT[:, c, t * 128:(t + 1) * 128], rhs=Wp[:, c, :], start=(c == 0), stop=(c == 7)),
                                reads=[tXT[t], tW[n]], writes=[tPS[ba]])
                        for c in range(2):
                            P.op("pe", lambda e, c=c, t=t, bb=bb, n=n: e.matmul(
                                PS[bb][:], lhsT=pT[:, c, t * 128:(t + 1) * 128], rhs=Wpp[:, c, n * 512:(n + 1) * 512],
                                start=(c == 0), stop=(c == 1)), reads=[tpT[t], tW[2]], writes=[tPS[bb]])
                        P.op("act", lambda e, ba=ba, si=si: e.activation(out=SGF[si], in_=PS[ba][:], func=AF.Sigmoid),
                             reads=[tPS[ba]], writes=[tSGF[si]])
                        P.op("dve", lambda e, bb=bb, si=si: e.tensor_tensor(out=SGF[si], in0=SGF[si], in1=PS[bb][:], op=ALU.mult),
                             reads=[tSGF[si], tPS[bb]], writes=[tSGF[si]])
                        P.op("pool", lambda e, t=t, n=n, si=si: e.tensor_tensor(
                            out=X[:, t, n * 512:(n + 1) * 512], in0=X[:, t, n * 512:(n + 1) * 512], in1=SGF[si], op=ALU.add),
                            reads=[tSGF[si], tX[t]], writes=[tX[t]])
                    if last:
                        P.dma("sp", y_d[t * 128:(t + 1) * 128, :], X[:, t, :], reads=[tX[t]])
                    else:
                        x_to_xt(t, 6, 1)

            NG = int(os.environ.get("KNG", "2"))
            GS_ = NT // NG
            grps = [list(range(GS_ * g, GS_ * g + GS_)) for g in range(NG)]
            P.begin_capture()
            A(grps[0])
            la = P.end_capture()
            P.begin_capture()
            pprep()
            lb = P.end_capture()
            P.replay([la, lb])
            for g in range(1, NG):
                P.begin_capture()
                A(grps[g])
                la = P.end_capture()
                P.begin_capture()
                B(grps[g - 1])
                lb = P.end_capture()
                P.replay([la, lb])
            B(grps[NG - 1])

        def out_proj_ln1(l, ychunk):
            d = Ld[l]
            load_w(0, d["w_out"][:, 0:512], 8)
            load_w(1, d["w_out"][:, 512:1024], 8)
            P.barrier()
            load_lnp(l, 0)
            Wo = [wslot(0, 8), wslot(1, 8)]
            kk = [0]

            def OP(grp):
                for t in grp:
                    for n in range(2):
                        b = kk[0] % 4
                        kk[0] += 1
                        for c in range(8):
                            src, tsrc = ychunk(c)
                            P.op("pe", lambda e, c=c, t=t, n=n, b=b, src=src: e.matmul(
                                PS[b][:], lhsT=src[:, t * 128:(t + 1) * 128], rhs=Wo[n][:, c, :], start=(c == 0), stop=(c == 7)),
                                reads=[tsrc[t], tW[n]], writes=[tPS[b]])
                        P.op("dve", lambda e, t=t, n=n, b=b: e.scalar_tensor_tensor(
                            out=X[:, t, n * 512:(n + 1) * 512], in0=X[:, t, n * 512:(n + 1) * 512], scalar=ALPHA, in1=PS[b][:],
                            op0=ALU.mult, op1=ALU.add), reads=[tPS[b], tX[t]], writes=[tX[t]])

            def LNX(grp):
                ln_group(grp, 0)
                for t in grp:
                    x_to_xt(t, 4 + t % 2)

            NG = int(os.environ.get("KNG", "2"))
            GS_ = NT // NG
            grps = [list(range(GS_ * g, GS_ * g + GS_)) for g in range(NG)]
            OP(grps[0])
            for g in range(1, NG):
                P.begin_capture()
                OP(grps[g])
                la = P.end_capture()
                P.begin_capture()
                LNX(grps[g - 1])
                lb = P.end_capture()
                P.replay([la, lb])
            LNX(grps[NG - 1])

        def layer_odd(l):
            d = Ld[l]
            P.barrier()
            ARENA.reset()
            QT = ARENA.take(4 * S * 2).rearrange("p (c f) -> p c f", c=4)
            KT = ARENA.take(4 * S * 2).rearrange("p (c f) -> p c f", c=4)
            V = ARENA.take(16 * 8 * 65 * 2).rearrange("p (t h e) -> p t h e", t=16, h=8)
            tQ = [T("q%d" % i) for i in range(4)]
            tK = [T("k%d" % i) for i in range(4)]
            tV = [T("v%d" % i) for i in range(NT)]
            CUM = LNP[:, 0:256]
            OFF = LNP[:, 256:528]
            TOT = LNP[:, 528:784]
            Z = LNP[:, 784:1040]
            BFB = LNP[:, 1040:1056]
            WF32 = LNP[:, 1056:1184].rearrange("p (c f) -> p c f", c=8)
            UT = LNP[:, 1184:1312]
            ONES = LNP[:, 1312:1440]
            YB = LNP[:, 1440:1952].bitcast(BF16).rearrange("p (a f) -> p a f", a=2)
            WFb = SM[:, 256:320].bitcast(BF16).rearrange("p (c f) -> p c f", c=8)
            tF = T("forget")
            P.dma("sp", WF32, d["w_in"][:, 3072:3088].rearrange("(c p) f -> p c f", p=128), writes=[tF, tLNP])
            P.dma("sp", BFB, d["bf"].broadcast_to([128, 16]), writes=[tF])
            P.op("dve", lambda e: e.tensor_copy(out=WFb, in_=WF32), reads=[tF], writes=[tF])
            P.op("dve", lambda e: e.memset(ONES, 1.0), writes=[tF])
            P.op("dve", lambda e: e.memset(UT, 1.0), writes=[tF])
            P.op("pool", lambda e: e.affine_select(out=UT, in_=UT, pattern=[[1, 128]], compare_op=ALU.is_ge, fill=0.0,
                                                   base=0, channel_multiplier=-1), reads=[tF], writes=[tF])
            for t in range(NT):
                for c in range(8):
                    P.op("pe", lambda e, t=t, c=c: e.matmul(PS[7][:, t * 16:(t + 1) * 16], lhsT=XT[:, c, t * 128:(t + 1) * 128],
                                                         rhs=WFb[:, c, :], start=(c == 0), stop=(c == 7)),
                         reads=[tXT[t], tF], writes=[tPS[7]])
            Z3 = Z.rearrange("p (t h) -> p t h", t=16)
            P.op("dve", lambda e: e.tensor_tensor(out=Z3, in0=PS[7][:, 0:256].rearrange("p (t h) -> p t h", t=16),
                                                  in1=BFB.unsqueeze(1).broadcast_to([128, 16, 16]), op=ALU.add),
                 reads=[tPS[7], tF], writes=[tF])
            P.op("act", lambda e: e.activation(out=Z, in_=Z, func=AF.Exp, scale=-1.0), reads=[tF], writes=[tF])
            P.op("act", lambda e: e.activation(out=Z, in_=Z, func=AF.Ln, bias=1.0), reads=[tF], writes=[tF])
            P.op("pe", lambda e: e.matmul(PS[6][:, 0:256], lhsT=UT, rhs=Z, start=True, stop=True), reads=[tF], writes=[tPS[6]])
            P.op("pe", lambda e: e.matmul(PS[7][:, 256:512], lhsT=ONES, rhs=Z, start=True, stop=True), reads=[tF], writes=[tPS[7]])
            P.op("dve", lambda e: e.tensor_copy(out=TOT, in_=PS[7][:, 256:512]), reads=[tPS[7]], writes=[tF])
            OFF3 = OFF.rearrange("p (t h) -> p t h", t=17)
            TOT3 = TOT.rearrange("p (t h) -> p t h", t=16)
            P.op("dve", lambda e: e.memset(OFF3[:, 0, :], 0.0), writes=[tF])
            for t in range(NT):
                P.op("dve", lambda e, t=t: e.tensor_tensor(out=OFF3[:, t + 1, :], in0=OFF3[:, t, :], in1=TOT3[:, t, :], op=ALU.add),
                     reads=[tF], writes=[tF])
            P.op("dve", lambda e: e.tensor_tensor(out=CUM, in0=PS[6][:, 0:256], in1=OFF[:, 0:256], op=ALU.add),
                 reads=[tPS[6], tF], writes=[tF])
            CUM3 = CUM.rearrange("p (t h) -> p t h", t=16)
            BIAS = SM[:, 320:384]
            tB = [T("b%d" % i) for i in range(4)]

            for half in range(2):
                if half == 1:
                    P.barrier()
                load_w(0, d["w_in"][:, half * 512:half * 512 + 512], 8)
                load_w(1, d["w_in"][:, 1024 + half * 512:1024 + half * 512 + 512], 8)
                load_w(2, d["w_in"][:, 2048 + half * 512:2048 + half * 512 + 512], 8)
                proj_feat(0, QT, tQ, 4, [0, 1, 2, 3])
                proj_feat(1, KT, tK, 4, [0, 1, 2, 3])
                P.op("dve", lambda e: e.memset(V[:, :, :, 64:65], 1.0), writes=tV)
                for t in range(NT):
                    b = 4 + t % 4
                    proj_tok(2, t, b)
                    evac(V[:, t, :, 0:64], PS[b][:].rearrange("p (h e) -> p h e", h=8), [tPS[b]], [tV[t]])
                if half == 1:
                    P.barrier()

                bk = [0, 0]
                cur_bias = {}

                def bias_fn(u, g, j):
                    return cur_bias[u][0][:, j:j + 1], [cur_bias[u][1]]

                def near_fn(u, g, j, ilo, nb, sap, st_, pap, pt_, stream):
                    if j == 0:
                        k = 2 * stream + bk[stream] % 2
                        bk[stream] += 1
                        hg = half * 8 + u
                        ap = BIAS[:, k * 16:(k + 1) * 16]
                        P.op("dve", lambda e, ap=ap, hg=hg, g=g: e.tensor_scalar(
                            out=ap, in0=CUM3[:, :, hg], scalar1=OFF3[:, 2 * g + 1, hg:hg + 1], scalar2=None,
                            op0=ALU.subtract), reads=[tF], writes=[tB[k]])
                        cur_bias[u] = (ap, tB[k])
                    return 0

                def post_exp_mask(j, ilo, pt):
                    pass

                def epilogue(u, g, acc, acc_t):
                    k = u % 2
                    rs = SM[:, 384 + 2 * (u % 8):384 + 2 * (u % 8) + 2]
                    trs = tRS[u % 8]
                    for ii in range(2):
                        P.op("dve", lambda e, ii=ii, rs=rs: e.reciprocal(out=rs[:, ii:ii + 1], in_=acc(ii)[:, 64:65]),
                             reads=[acc_t(ii)], writes=[trs])
                        P.op("dve", lambda e, ii=ii, rs=rs, u=u: e.tensor_scalar(
                            out=YB[:, ii, u * 64:(u + 1) * 64], in0=acc(ii)[:, 0:64], scalar1=rs[:, ii:ii + 1], scalar2=None,
                            op0=ALU.mult), reads=[acc_t(ii), trs], writes=[tYB[ii]])

                tRS = [T("rs%d" % i) for i in range(8)]
                tYB = [T("yb0"), T("yb1")]

                def after_group(g):
                    for ii in range(2):
                        t = 2 * g + ii
                        if half == 0:
                            transposes(lambda k, ii=ii: YB[:, ii, k * 128:(k + 1) * 128], 4, [tYB[ii]], 3 * ii,
                                       YTb[:, :, t * 128:(t + 1) * 128], [tYTb[t]])
                        else:
                            transposes(lambda k, ii=ii: YB[:, ii, k * 128:(k + 1) * 128], 4, [tYB[ii]], 3 * ii,
                                       XT[:, 0:4, t * 128:(t + 1) * 128], [tXT[t]])

                attention(8, lambda u: 64 * (u % 2), lambda u: u // 2, lambda u: u, QT, KT, tQ, tK, V, tV, 65, 2,
                          bias_fn, near_fn, epilogue, after_group, lambda u, i: (6 + u % 2, i * 65), True, look=2)

            def ychunk(c):
                if c < 4:
                    return YTb[:, c, :], tYTb
                return XT[:, c - 4, :], tXT
            out_proj_ln1(l, ychunk)


        def layer_even(l):
            d = Ld[l]
            lam_init = 0.8 - 0.6 * math.exp(-0.3 * l)
            if not xt_pending:
                P.barrier()
            ARENA.reset()
            tC = T("consts")
            CT = ARENA.take(NCONST * 4, F32)
            P.dma("sp", CT, consts_d, writes=[tC])
            COS = CT[:, C_COS:C_COS + 512].rearrange("p (t f) -> p t f", t=16)
            SIN = CT[:, C_SIN:C_SIN + 512].rearrange("p (t f) -> p t f", t=16)
            DM = CT[:, C_DM:C_DM + 512].rearrange("p (h c) -> p h c", h=4)
            QD = CT[:, C_QD:C_QD + 4]
            KD = CT[:, C_KD:C_KD + 4]
            RG = ARENA.take(128 * 4, F32)
            P.dma("sp", RG, d["rg"].broadcast_to([128, 128]), writes=[tC])
            load_w(0, d["w_in"][:, 1536:2048], 8)
            load_w(1, d["w_in"][:, 2048:2560], 8)
            load_w(2, d["w_in"][:, 2560:3072], 8)
            ST32 = ARENA.take(512 * 4, F32).rearrange("p (b e) -> p b e", b=4)
            STB = ARENA.take(512 * 2).rearrange("p (b e) -> p b e", b=4)
            KZ = [ARENA.take(512 * 2).rearrange("p (b e) -> p b e", b=4) for _ in range(2)]
            tKZ = [T("kz0"), T("kz1")]
            for k_ in range(2):
                P.op("dve", lambda e, k_=k_: e.memset(KZ[k_], 0.0), writes=[tKZ[k_]])
            tST = T("st32")
            tSTB = T("stb")
            P.op("dve", lambda e: e.memset(ST32, 0.0), writes=[tST])

            def two(nbytes, dt=BF16):
                return [ARENA.take(nbytes, dt) for _ in range(2)]
            T1a = two(1024, F32); T2a = two(1024, F32); T1b = two(1024, F32); T2b = two(1024, F32)
            QKR = two(2048, F32); QKS = two(1024); KRD = two(512); QKT = two(1024); VB = two(1024)
            GS = two(2048, F32); SCT = two(1024); OSB = two(2048, F32); YN = two(2048, F32); YBt = two(1024)
            STT = two(24 * 4, F32); MV = two(16 * 4, F32)
            names = "t1a t2a t1b t2b qkr qks krd qrt krt vb gs sct osb yn ybt stt".split()
            tt_ = {n: [T(n + "0"), T(n + "1")] for n in names}
            def ret_front(i):
                k = i % 2
                t = i
                proj_tok(0, t, 0)
                qk = PS[0][:].rearrange("p (h a f) -> p h a f", h=8, a=2)
                qkr = QKR[k].rearrange("p (h a f) -> p h a f", h=8, a=2)
                cosb = COS[:, t, :].unsqueeze(1).broadcast_to([128, 8, 32])
                sinb = SIN[:, t, :].unsqueeze(1).broadcast_to([128, 8, 32])
                v3 = lambda ap: ap.rearrange("p (h f) -> p h f", h=8)
                P.op("dve", lambda e, k=k, qk=qk, cosb=cosb: e.tensor_tensor(out=v3(T1a[k]), in0=qk[:, :, 0, :], in1=cosb, op=ALU.mult),
                     reads=[tPS[0], tC], writes=[tt_["t1a"][k]])
                P.op("dve", lambda e, k=k, qk=qk, sinb=sinb: e.tensor_tensor(out=v3(T2a[k]), in0=qk[:, :, 1, :], in1=sinb, op=ALU.mult),
                     reads=[tPS[0], tC], writes=[tt_["t2a"][k]])
                P.op("dve", lambda e, k=k, qk=qk, cosb=cosb: e.tensor_tensor(out=v3(T1b[k]), in0=qk[:, :, 1, :], in1=cosb, op=ALU.mult),
                     reads=[tPS[0], tC], writes=[tt_["t1b"][k]])
                P.op("dve", lambda e, k=k, qk=qk, sinb=sinb: e.tensor_tensor(out=v3(T2b[k]), in0=qk[:, :, 0, :], in1=sinb, op=ALU.mult),
                     reads=[tPS[0], tC], writes=[tt_["t2b"][k]])
                P.op("dve", lambda e, k=k, qkr=qkr: e.tensor_tensor(out=qkr[:, :, 0, :], in0=v3(T1a[k]), in1=v3(T2a[k]), op=ALU.subtract),
                     reads=[tt_["t1a"][k], tt_["t2a"][k]], writes=[tt_["qkr"][k]])
                P.op("dve", lambda e, k=k, qkr=qkr: e.tensor_tensor(out=qkr[:, :, 1, :], in0=v3(T1b[k]), in1=v3(T2b[k]), op=ALU.add),
                     reads=[tt_["t1b"][k], tt_["t2b"][k]], writes=[tt_["qkr"][k]])
                proj_tok(1, t, 1)
                evac(VB[k], PS[1][:], [tPS[1]], [tt_["vb"][k]])
                proj_tok(2, t, 2)
                P.op("act", lambda e, k=k: e.activation(out=GS[k], in_=PS[2][:], func=AF.Silu), reads=[tPS[2]], writes=[tt_["gs"][k]])
                P.op("dve", lambda e, k=k: e.tensor_tensor(out=QKS[k][:, 0:256].rearrange("p (h f) -> p h f", h=4),
                                                           in0=QKR[k][:, 0:256].rearrange("p (h f) -> p h f", h=4),
                                                           in1=QD.unsqueeze(2).broadcast_to([128, 4, 64]), op=ALU.mult),
                     reads=[tt_["qkr"][k], tC], writes=[tt_["qks"][k]])
                P.op("dve", lambda e, k=k: e.tensor_tensor(out=KRD[k].rearrange("p (h f) -> p h f", h=4),
                                                           in0=QKR[k][:, 256:512].rearrange("p (h f) -> p h f", h=4),
                                                           in1=KD.unsqueeze(2).broadcast_to([128, 4, 64]), op=ALU.mult),
                     reads=[tt_["qkr"][k], tC], writes=[tt_["krd"][k]])
                P.op("pool", lambda e, k=k: e.tensor_scalar(out=QKS[k][:, 256:512], in0=QKR[k][:, 256:512], scalar1=0.125, scalar2=None,
                                                            op0=ALU.mult), reads=[tt_["qkr"][k]], writes=[tt_["qks"][k]])
                transposes(lambda b, k=k: QKS[k][:, b * 128:(b + 1) * 128], 4, [tt_["qks"][k]], 3,
                           QKT[k].rearrange("p (b c) -> p b c", b=4), [tt_["qrt"][k]])
                for h in range(4):
                    hp, hb = 64 * (h % 2), h // 2
                    P.op("dve", lambda e, h=h, hp=hp, hb=hb, k=k: e.tensor_copy(
                        out=KZ[k][hp:hp + 64, h, :], in_=QKT[k][hp:hp + 64, (2 + hb) * 128:(3 + hb) * 128]),
                        reads=[tt_["qrt"][k]], writes=[tKZ[k]])

            def ret_back(i):
                k = i % 2
                t = i
                qrt = QKT[k][:, 0:256].rearrange("p (b c) -> p b c", b=2)
                krt = QKT[k][:, 256:512].rearrange("p (b c) -> p b c", b=2)
                for h in range(4):
                    hp, hb = 64 * (h % 2), h // 2
                    P.op("pe", lambda e, h=h, hb=hb, k=k, qrt=qrt: e.matmul(
                        PS[4][:, h * 128:(h + 1) * 128], lhsT=KZ[k][:, h, :], rhs=qrt[:, hb, :], start=True, stop=True),
                        reads=[tKZ[k], tt_["qrt"][k]], writes=[tPS[4]])
                P.op("dve", lambda e, k=k: e.tensor_tensor(out=SCT[k].rearrange("p (h c) -> p h c", h=4),
                                                           in0=PS[4][:].rearrange("p (h c) -> p h c", h=4), in1=DM, op=ALU.mult),
                     reads=[tPS[4], tC], writes=[tt_["sct"][k]])
                sct = SCT[k].rearrange("p (h c) -> p h c", h=4)
                for h in range(4):
                    hp, hb = 64 * (h % 2), h // 2
                    P.op("pe", lambda e, h=h, sct=sct, k=k, i=i: e.matmul(
                        PS[5][:, h * 128:(h + 1) * 128], lhsT=sct[:, h, :], rhs=VB[k][:, h * 128:(h + 1) * 128], start=True, stop=(i == 0)),
                        reads=[tt_["sct"][k], tt_["vb"][k]], writes=[tPS[5]])
                    if i > 0:
                        P.op("pe", lambda e, h=h, hb=hb, qrt=qrt: e.matmul(
                            PS[5][:, h * 128:(h + 1) * 128], lhsT=qrt[:, hb, :], rhs=STB[:, h, :], start=False, stop=True),
                            reads=[tt_["qrt"][k], tSTB], writes=[tPS[5]])
                if i < NT - 1:
                    for hb in range(2):
                        P.op("pe", lambda e, hb=hb, k=k: e.matmul(
                            PS[6][:, hb * 256:(hb + 1) * 256], lhsT=KRD[k][:, hb * 128:(hb + 1) * 128], rhs=VB[k][:, hb * 256:(hb + 1) * 256],
                            start=True, stop=True), reads=[tt_["krd"][k], tt_["vb"][k]], writes=[tPS[6]])
                    for h in range(4):
                        hp, hb = 64 * (h % 2), h // 2
                        P.op("dve", lambda e, h=h, hp=hp, hb=hb: e.scalar_tensor_tensor(
                            out=ST32[hp:hp + 64, h, :], in0=ST32[hp:hp + 64, h, :], scalar=CD[h],
                            in1=PS[6][hp:hp + 64, hb * 256 + (h % 2) * 128:hb * 256 + (h % 2) * 128 + 128], op0=ALU.mult, op1=ALU.add),
                            reads=[tPS[6], tST], writes=[tST])
                    P.op("act", lambda e: e.copy(out=STB, in_=ST32), reads=[tST], writes=[tSTB])
                P.op("act", lambda e, k=k: e.copy(out=OSB[k], in_=PS[5][:]), reads=[tPS[5]], writes=[tt_["osb"][k]])
                stt = STT[k].rearrange("p (h s) -> p h s", h=4)
                mv = MV[k].rearrange("p (h s) -> p h s", h=4)
                tS = tt_["stt"][k]
                for h in range(4):
                    P.op("dve", lambda e, h=h, k=k, stt=stt: e.bn_stats(out=stt[:, h, :], in_=OSB[k][:, h * 128:(h + 1) * 128]),
                         reads=[tt_["osb"][k]], writes=[tS])
                for h in range(4):
                    P.op("dve", lambda e, h=h, stt=stt, mv=mv: e.bn_aggr(out=mv[:, h, 0:2], in_=stt[:, h, :]), reads=[tS], writes=[tS])
                P.op("act", lambda e, mv=mv: e.activation(out=mv[:, :, 2], in_=mv[:, :, 1], func=AF.Sqrt, bias=EPSB[:, 0:1]),
                     reads=[tS, tid], writes=[tS])
                P.op("dve", lambda e, mv=mv: e.reciprocal(out=mv[:, :, 2], in_=mv[:, :, 2]), reads=[tS], writes=[tS])
                P.op("dve", lambda e, mv=mv: e.scalar_tensor_tensor(out=mv[:, :, 3], in0=mv[:, :, 0], scalar=-1.0, in1=mv[:, :, 2],
                                                                    op0=ALU.mult, op1=ALU.mult), reads=[tS], writes=[tS])
                for h in range(4):
                    P.op("act", lambda e, h=h, k=k, mv=mv: e.activation(out=YN[k][:, h * 128:(h + 1) * 128], in_=OSB[k][:, h * 128:(h + 1) * 128],
                                                                        func=AF.Identity, scale=mv[:, h, 2:3], bias=mv[:, h, 3:4]),
                         reads=[tt_["osb"][k], tS], writes=[tt_["yn"][k]])
                P.op("dve", lambda e, k=k: e.tensor_tensor(out=GS[k].rearrange("p (h c) -> p h c", h=4),
                                                           in0=GS[k].rearrange("p (h c) -> p h c", h=4),
                                                           in1=RG.unsqueeze(1).broadcast_to([128, 4, 128]), op=ALU.mult),
                     reads=[tt_["gs"][k], tC], writes=[tt_["gs"][k]])
                P.op("dve", lambda e, k=k: e.tensor_tensor(out=YBt[k], in0=YN[k], in1=GS[k], op=ALU.mult),
                     reads=[tt_["yn"][k], tt_["gs"][k]], writes=[tt_["ybt"][k]])
                transposes(lambda kk, k=k: YBt[k][:, kk * 128:(kk + 1) * 128], 4, [tt_["ybt"][k]], 7,
                           YTb[:, :, t * 128:(t + 1) * 128], [tYTb[t]])


            for s_ in range(NT + 1):
                lists = []
                if xt_pending:
                    t_ = xt_pending.pop(0)
                    x_to_xt(t_, t_ % 2)
                if s_ < NT:
                    P.begin_capture()
                    ret_front(s_)
                    lists.append(P.end_capture())
                if s_ >= 1:
                    P.begin_capture()
                    ret_back(s_ - 1)
                    lists.append(P.end_capture())
                P.replay(lists)

            P.barrier()
            ARENA.reset()
            QT = ARENA.take(4 * S * 2).rearrange("p (c f) -> p c f", c=4)
            KT = ARENA.take(4 * S * 2).rearrange("p (c f) -> p c f", c=4)
            V = ARENA.take(16 * 4 * 129 * 2).rearrange("p (t h e) -> p t h e", t=16, h=4)
            tQ = [T("q%d" % i) for i in range(4)]
            tK = [T("k%d" % i) for i in range(4)]
            tV = [T("v%d" % i) for i in range(NT)]
            load_w(0, d["w_in"][:, 0:512], 8)
            load_w(1, d["w_in"][:, 512:1024], 8)
            load_w(2, d["w_in"][:, 1024:1536], 8)
            proj_feat(0, QT, tQ, 4, [0, 1, 2, 3])
            proj_feat(1, KT, tK, 4, [0, 1, 2, 3])
            P.op("dve", lambda e: e.memset(V[:, :, :, 128:129], 1.0), writes=tV)
            for t in range(NT):
                b = 4 + t % 4
                proj_tok(2, t, b)
                evac(V[:, t, :, 0:128], PS[b][:].rearrange("p (h e) -> p h e", h=4), [tPS[b]], [tV[t]])
            P.barrier()
            RELB = LNP[:, 0:1024].rearrange("p (h j) -> p h j", h=4)
            YB4 = LNP[:, 1024:2048].bitcast(BF16).rearrange("p (a f) -> p a f", a=4)
            HF = HB[:, 1, :].bitcast(F32)
            LAM32 = HF[:, 0:256]
            DGB = HF[:, 256:384]
            LS = HF[:, 384:386]
            NL = HF[:, 386:387]
            CH = HF[:, 388:392]
            tL = T("lam")
            tR = T("relb")
            P.dma("sp", LAM32, d["lam"].broadcast_to([128, 256]), writes=[tL])
            P.dma("sp", DGB, d["dg"].broadcast_to([128, 128]), writes=[tL])
            P.dma("sp", LNP[:, 0:1024], d["relbT"], writes=[tR, tLNP])
            lamv = LAM32.rearrange("p (a b f) -> p a b f", a=2, b=2)
            P.op("dve", lambda e: e.tensor_tensor(out=lamv[:, :, 0, :], in0=lamv[:, :, 0, :], in1=lamv[:, :, 1, :], op=ALU.mult),
                 reads=[tL], writes=[tL])
            P.op("dve", lambda e: e.tensor_reduce(out=LS, in_=lamv[:, :, 0, :], axis=AX.X, op=ALU.add), reads=[tL], writes=[tL])
            P.op("act", lambda e: e.activation(out=LS, in_=LS, func=AF.Exp), reads=[tL], writes=[tL])
            P.op("dve", lambda e: e.tensor_scalar(out=NL, in0=LS[:, 1:2], scalar1=LS[:, 0:1], scalar2=-lam_init,
                                                  op0=ALU.subtract, op1=ALU.add), reads=[tL], writes=[tL])
            P.op("dve", lambda e: e.tensor_scalar(out=DGB, in0=DGB, scalar1=1.0 - lam_init, scalar2=None, op0=ALU.mult),
                 reads=[tL], writes=[tL])
            P.op("dve", lambda e: e.tensor_copy(out=CH, in_=RELB[:, :, 255]), reads=[tR], writes=[tL])
            for h in range(4):
                P.op("dve", lambda e, h=h: e.tensor_scalar(out=RELB[:, h, :], in0=RELB[:, h, :], scalar1=CH[:, h:h + 1], scalar2=None,
                                                           op0=ALU.subtract), reads=[tR, tL], writes=[tR])
            P.op("pool", lambda e: e.affine_select(out=RELB, in_=RELB, pattern=[[0, 4], [1, 256]], compare_op=ALU.is_ge, fill=-30000.0,
                                                   base=0, channel_multiplier=-1), reads=[tR], writes=[tR])
            EPF = HB[:, 0, :].bitcast(F32)
            tEP = [T("ep0"), T("ep1")]
            tYB4 = [T("yb4%d" % i) for i in range(4)]
            epk = [0]
            btk = [0, 0]

            def bias_fn(u, g, j):
                return 0.0, []

            def near_fn(u, g, j, ilo, nb, sap, st_, pap, pt_, stream):
                h = u // 2
                if ilo == j:
                    nn, boff = min(2, nb), 0
                elif ilo == j + 1:
                    nn, boff = 1, 128
                else:
                    return 0
                k = 2 * stream + btk[stream] % 2
                btk[stream] += 1
                P.op("dve", lambda e: e.scalar_tensor_tensor(out=BT[:, k, 0:nn * 128], in0=sap[:, 0:nn * 128], scalar=0.125,
                                                             in1=RELB[:, h, boff:boff + nn * 128], op0=ALU.mult, op1=ALU.add),
                     reads=[st_, tR], writes=[tBT[k]])
                P.op("act", lambda e: e.activation(out=pap[:, 0:nn * 128], in_=BT[:, k, 0:nn * 128], func=AF.Exp),
                     reads=[tBT[k]], writes=[pt_])
                return nn

            def acc_map(u, i):
                c = u % 2
                if i < 3:
                    return 4 + 2 * c, i * 129
                return 5 + 2 * c, 0

            def epilogue(u, g, acc, acc_t):
                if u % 2 == 0:
                    return
                h = u // 2
                for i in range(4):
                    k = epk[0] % 2
                    epk[0] += 1
                    b1, o1 = acc_map(u - 1, i)
                    b2, o2 = acc_map(u, i)
                    O1 = PS[b1][:, o1:o1 + 129]
                    O2 = PS[b2][:, o2:o2 + 129]
                    TA = EPF[:, k * 256:k * 256 + 128]
                    YA = EPF[:, k * 256 + 128:k * 256 + 256]
                    RS = SM[:, 400 + k * 8:400 + k * 8 + 8]
                    te = tEP[k]
                    P.op("dve", lambda e, O1=O1, RS=RS: e.reciprocal(out=RS[:, 0:1], in_=O1[:, 128:129]), reads=[tPS[b1]], writes=[te])
                    P.op("dve", lambda e, O2=O2, RS=RS: e.reciprocal(out=RS[:, 1:2], in_=O2[:, 128:129]), reads=[tPS[b2]], writes=[te])
                    P.op("dve", lambda e, RS=RS: e.tensor_tensor(out=RS[:, 2:3], in0=RS[:, 1:2], in1=NL, op=ALU.mult), reads=[te, tL], writes=[te])
                    P.op("dve", lambda e, O2=O2, RS=RS, TA=TA: e.tensor_scalar(out=TA, in0=O2[:, 0:128], scalar1=RS[:, 2:3], scalar2=None,
                                                                               op0=ALU.mult), reads=[tPS[b2], te], writes=[te])
                    P.op("dve", lambda e, O1=O1, RS=RS, TA=TA, YA=YA: e.scalar_tensor_tensor(
                        out=YA, in0=O1[:, 0:128], scalar=RS[:, 0:1], in1=TA, op0=ALU.mult, op1=ALU.add), reads=[tPS[b1], te], writes=[te])
                    P.op("act", lambda e, RS=RS, TA=TA, YA=YA: e.activation(out=TA, in_=YA, func=AF.Square, accum_out=RS[:, 3:4]),
                         reads=[te], writes=[te])
                    P.op("act", lambda e, RS=RS: e.activation(out=RS[:, 4:5], in_=RS[:, 3:4], func=AF.Sqrt, scale=1.0 / 128.0, bias=EPSB[:, 0:1]),
                         reads=[te, tid], writes=[te])
                    P.op("dve", lambda e, RS=RS: e.reciprocal(out=RS[:, 4:5], in_=RS[:, 4:5]), reads=[te], writes=[te])
                    P.op("dve", lambda e, RS=RS, YA=YA, i=i, h=h: e.scalar_tensor_tensor(
                        out=YB4[:, i, h * 128:(h + 1) * 128], in0=YA, scalar=RS[:, 4:5], in1=DGB, op0=ALU.mult, op1=ALU.mult),
                        reads=[te, tL], writes=[tYB4[i]])

            def after_group(g):
                for ii in range(4):
                    t = 4 * g + ii
                    transposes(lambda kk, ii=ii: YB4[:, ii, kk * 128:(kk + 1) * 128], 4, [tYB4[ii]], ii % 2,
                               XT[:, 0:4, t * 128:(t + 1) * 128], [tXT[t]])

            attention(8, lambda u: 64 * (u % 2), lambda u: u // 2, lambda u: u // 2, QT, KT, tQ, tK, V, tV, 129, 4,
                      bias_fn, near_fn, epilogue, after_group, acc_map, False)

            def ychunk(c):
                if c < 4:
                    return XT[:, c, :], tXT
                return YTb[:, c - 4, :], tYTb
            out_proj_ln1(l, ychunk)

        for li, l in enumerate(layers):
            if l == 1:
                layer_odd(l)
            else:
                layer_even(l)
            if STOP == "M":
                P.barrier()
                for t in range(NT):
                    P.dma("sp", y_d[t * 128:(t + 1) * 128, :], X[:, t, :], reads=[tX[t]])
                break
            ffn_block(l, li == len(layers) - 1)
            continue
            if STOP:
                P.barrier()
                for t in range(NT):
                    P.dma("sp", y_d[t * 128:(t + 1) * 128, :], X[:, t, :], reads=[tX[t]])
                break
            ffn_block(l, li == len(layers) - 1)

        P.emit()
    return nc


_NC_CACHE = {}


def _get_nc(layers):
    key = tuple(layers)
    if key not in _NC_CACHE:
        _NC_CACHE[key] = build(list(layers))
    return _NC_CACHE[key]


def _layer_inputs(l, b, inp):
    f = lambda a: np.ascontiguousarray(a, dtype=np.float32)
    d = {}
    j = l // 2
    d["p%d" % l] = f(inp["p"][l, b])
    d["lnp%d" % l] = f(np.stack([inp["ln_mix_g"][l], inp["ln_mix_b"][l], inp["ln_ffn_g"][l], inp["ln_ffn_b"][l]]))
    d["wg%d" % l] = f(inp["moe_w_gate"][l])
    d["wu%d" % l] = f(inp["moe_w_up"][l])
    d["wd%d" % l] = f(inp["moe_w_down"][l])
    d["pproj%d" % l] = f(inp["ple_proj"][l])
    d["pgate%d" % l] = f(inp["ple_gate"][l])
    if l % 2 == 0:
        d["w_in%d" % l] = f(inp["even_w_in"][j])
        d["w_out%d" % l] = f(inp["even_w_out"][j])
        pp = np.arange(128)[:, None]
        jj = np.arange(256)[None, :]
        bucket = _t5_bucket_np(np.maximum(jj - pp, 0))
        rb = np.asarray(inp["rel_bias"], np.float32)
        g = rb[bucket]
        d["relbT"] = f(np.transpose(g, (0, 2, 1)).reshape(128, 1024))
        d["lam"] = f(np.asarray(inp["even_lambda"][j]).reshape(1, 256))
        d["dg"] = f(np.asarray(inp["even_diff_norm"][j]).reshape(1, 128))
        d["rg"] = f(np.asarray(inp["even_ret_norm"][j]).reshape(1, 128))
    else:
        d["w_in%d" % l] = f(inp["odd_w_in"][j])
        d["w_out%d" % l] = f(inp["odd_w_out"][j])
        d["bf"] = f(np.asarray(inp["odd_b_forget"][j]).reshape(1, 16))
    return d


def run_layers(layers, xin, inp):
    nc = _get_nc(layers)
    consts, _ = _static_consts()
    in_maps = []
    for b in range(8):
        m = {"x": np.ascontiguousarray(xin[b], dtype=np.float32), "consts": consts,
             "router_w": np.ascontiguousarray(inp["router_w"], dtype=np.float32)}
        for l in layers:
            m.update(_layer_inputs(l, b, inp))
        in_maps.append(m)
    res = run_bass_kernel_spmd(nc, in_maps, core_ids=list(range(8)))
    return np.stack([np.asarray(r["y"], dtype=np.float32) for r in res.results], axis=0)


def kernel(**inputs):
    inp = {k: np.asarray(v) for k, v in inputs.items()}
    x = np.asarray(inp["x"], dtype=np.float32)
    x = run_layers([0, 1], x, inp)
    return x.astype(np.float32)
```

```python
import math
from contextlib import ExitStack

import numpy as np
import concourse.bass as bass
import concourse.mybir as mybir
from concourse.bass_utils import run_bass_kernel_spmd

F32 = mybir.dt.float32
BF16 = mybir.dt.bfloat16
AF = mybir.ActivationFunctionType
ALU = mybir.AluOpType
AX = mybir.AxisListType

S = 2048
D = 1024
NT = 16
ALPHA = float((2 * 2) ** 0.25)
EPS = 1e-5
DMA_RING = 8
COMPUTE = ("pe", "act", "dve", "pool")


class T:
    __slots__ = ("name", "last_w", "readers")

    def __init__(self, name=""):
        self.name = name
        self.last_w = None
        self.readers = []


class Instr:
    __slots__ = ("idx", "eng", "fn", "deps", "is_dma", "signal", "count", "ring", "ringval", "kq")

    def __init__(self, idx, eng, fn, is_dma):
        self.idx = idx
        self.eng = eng
        self.fn = fn
        self.deps = set()
        self.is_dma = is_dma
        self.signal = is_dma
        self.count = None
        self.ring = None
        self.ringval = None
        self.kq = None


class Prog:
    def __init__(self, nc):
        self.nc = nc
        self.instrs = []
        self.streams = {e: [] for e in ("pe", "act", "dve", "pool", "sp")}
        self.dma_count = {e: 0 for e in ("sp", "act", "pool")}
        self.pending = {}
        self.dmas_since_barrier = set()
        self.cap = None

    def begin_capture(self):
        self.cap = []

    def end_capture(self):
        lst, self.cap = self.cap, None
        return lst

    def replay(self, lists):
        idx = [0] * len(lists)
        left = sum(len(l) for l in lists)
        while left:
            for k, l in enumerate(lists):
                if idx[k] < len(l):
                    kind, a, b, c, r, w = l[idx[k]]
                    idx[k] += 1
                    left -= 1
                    if kind == "op":
                        self.op(a, b, r, w)
                    else:
                        self.dma(a, b, c, r, w)

    def barrier(self):
        deps = set(self.dmas_since_barrier)
        self.dmas_since_barrier = set()
        for e, st in self.streams.items():
            for ins in reversed(st):
                if not ins.is_dma:
                    deps.add(ins.idx)
                    break
        for e in self.streams:
            self.pending[e] = self.pending.get(e, set()) | deps

    def _add(self, eng, fn, reads, writes, is_dma):
        ins = Instr(len(self.instrs), eng, fn, is_dma)
        self.instrs.append(ins)
        self.streams[eng].append(ins)
        if eng in self.pending:
            ins.deps |= self.pending.pop(eng)
        for t in reads:
            if t.last_w is not None:
                ins.deps.add(t.last_w)
        for t in writes:
            if t.last_w is not None:
                ins.deps.add(t.last_w)
            for r in t.readers:
                ins.deps.add(r)
        for t in writes:
            t.last_w = ins.idx
            t.readers = []
        for t in reads:
            if t.last_w == ins.idx:
                continue
            if not is_dma:
                t.readers = [r for r in t.readers
                             if self.instrs[r].is_dma or self.instrs[r].eng != eng]
            t.readers.append(ins.idx)
        ins.deps.discard(ins.idx)
        if is_dma:
            self.dmas_since_barrier.add(ins.idx)
        return ins

    def op(self, eng, fn, reads=(), writes=()):
        if self.cap is not None:
            self.cap.append(("op", eng, fn, None, list(reads), list(writes)))
            return None
        return self._add(eng, fn, reads, writes, False)

    def dma(self, q, out, in_, reads=(), writes=()):
        if self.cap is not None:
            self.cap.append(("dma", q, out, in_, list(reads), list(writes)))
            return None

        def fn(e, out=out, in_=in_):
            return e.dma_start(out=out, in_=in_)
        ins = self._add(q, fn, reads, writes, True)
        ins.kq = self.dma_count[q]
        self.dma_count[q] += 1
        return ins

    def emit(self):
        nc = self.nc
        instrs = self.instrs
        for ins in instrs:
            nd = set()
            for d in ins.deps:
                di = instrs[d]
                if di.eng == "pe" and ins.eng == "pe" and not di.is_dma and not ins.is_dma:
                    continue
                nd.add(d)
            ins.deps = nd
            for d in nd:
                instrs[d].signal = True
        for e, st in self.streams.items():
            c = 0
            for ins in st:
                if ins.is_dma:
                    continue
                if ins.signal:
                    c += 1
                    ins.count = c
        with ExitStack() as es:
            sem_eng = {e: es.enter_context(nc.semaphore("s_" + e)) for e in COMPUTE}
            rings = {}
            for q in ("sp", "act", "pool"):
                if self.dma_count[q]:
                    rings[q] = [es.enter_context(nc.semaphore("r_%s%d" % (q, i)))
                                for i in range(DMA_RING)]
            for ins in instrs:
                if ins.is_dma:
                    ins.ring = rings[ins.eng][ins.kq % DMA_RING]
                    ins.ringval = 16 * (ins.kq // DMA_RING + 1)
            block = es.enter_context(nc.Block())

            def run_stream(ename, eobj):
                known = {}
                last_dma = {}
                for ins in self.streams[ename]:
                    waits = {}
                    for d in ins.deps:
                        di = instrs[d]
                        if di.is_dma:
                            sem, val = di.ring, di.ringval
                        else:
                            sem, val = sem_eng[di.eng], di.count
                        key = id(sem)
                        if known.get(key, 0) >= val:
                            continue
                        if key not in waits or waits[key][1] < val:
                            waits[key] = (sem, val)
                    if ins.is_dma and ins.kq >= DMA_RING:
                        sem = ins.ring
                        val = ins.ringval - 16
                        key = id(sem)
                        if known.get(key, 0) < val and (key not in waits or waits[key][1] < val):
                            waits[key] = (sem, val)
                    for key, (sem, val) in waits.items():
                        eobj.wait_ge(sem, val)
                        known[key] = val
                    bi = ins.fn(eobj)
                    if ins.is_dma:
                        bi.then_inc(ins.ring, 16)
                        last_dma[id(ins.ring)] = (ins.ring, ins.ringval)
                    elif ins.signal:
                        bi.then_inc(sem_eng[ename], 1)
                for key, (sem, val) in last_dma.items():
                    if known.get(key, 0) < val:
                        eobj.wait_ge(sem, val)

            if self.streams["sp"]:
                @block.sync
                def _(e):
                    run_stream("sp", e)
            if self.streams["pe"]:
                @block.tensor
                def _(e):
                    run_stream("pe", e)
            if self.streams["act"]:
                @block.scalar
                def _(e):
                    run_stream("act", e)
            if self.streams["dve"]:
                @block.vector
                def _(e):
                    run_stream("dve", e)
            if self.streams["pool"]:
                @block.gpsimd
                def _(e):
                    run_stream("pool", e)


def _t5_bucket_np(dist):
    d = np.maximum(dist, 1).astype(np.float32)
    large = 16 + (np.log(d / np.float32(16)) / np.float32(math.log(128 / 16)) * np.float32(16)).astype(np.int32)
    large = np.minimum(large, 31)
    return np.where(dist < 16, dist, large)


C_COS = 0
C_SIN = 512
C_DM = 1024
C_QD = 1536
C_KD = 1792
NCONST = 1800


def _static_consts():
    c = np.zeros((128, NCONST), np.float32)
    p = np.arange(128)
    half = 32
    inv = (10000.0 ** (-np.arange(half, dtype=np.float32) / half)).astype(np.float32)
    for t in range(NT):
        pos = (t * 128 + p).astype(np.float32)
        ang = pos[:, None] * inv[None, :]
        c[:, C_COS + t * 32:C_COS + (t + 1) * 32] = np.cos(ang)
        c[:, C_SIN + t * 32:C_SIN + (t + 1) * 32] = np.sin(ang)
    log_g = np.log1p(-np.exp2(-5.0 - np.arange(4, dtype=np.float64)))
    cc = np.arange(128)
    for h in range(4):
        dm = np.where(cc[None, :] >= p[:, None], np.exp(-(p[:, None] + 1.0) * log_g[h]), 0.0)
        c[:, C_DM + h * 128:C_DM + (h + 1) * 128] = dm
        c[:, C_KD + h] = np.exp((127.0 - p) * log_g[h]) * 0.125
    for h in range(4):
        c[:, C_QD + h] = np.exp((p + 1.0) * log_g[h])
    cd = [float(np.exp(128.0 * log_g[h])) for h in range(4)]
    return c, cd


import os
STOP = os.environ.get('KSTOP', '')
RET_N = int(os.environ.get('KRETN', '16'))
STAGE = int(os.environ.get('KSTAGE', '9'))


def build(layers, debug=None):
    nc = bass.Bass("TRN2", target_bir_lowering=False)
    _, CD = _static_consts()

    def din(name, shape):
        return nc.dram_tensor(name, list(shape), F32, kind="ExternalInput").ap()

    x_d = din("x", [S, D])
    consts_d = din("consts", [128, NCONST])
    router_d = din("router_w", [D, 16])
    Ld = {}
    for l in layers:
        d = {}
        d["p"] = din("p%d" % l, [S, 256])
        d["w_in"] = din("w_in%d" % l, [D, 3072 if l == 0 else 3088])
        d["w_out"] = din("w_out%d" % l, [D, D])
        d["lnp"] = din("lnp%d" % l, [4, D])
        d["wg"] = din("wg%d" % l, [16, D, 512])
        d["wu"] = din("wu%d" % l, [16, D, 512])
        d["wd"] = din("wd%d" % l, [16, 512, D])
        d["pproj"] = din("pproj%d" % l, [256, D])
        d["pgate"] = din("pgate%d" % l, [D, D])
        if l == 0:
            d["relbT"] = din("relbT", [128, 4 * 256])
            d["lam"] = din("lam", [1, 256])
            d["dg"] = din("dg", [1, 128])
            d["rg"] = din("rg", [1, 128])
        else:
            d["bf"] = din("bf", [1, 16])
        Ld[l] = d
    y_d = nc.dram_tensor("y", [S, D], F32, kind="ExternalOutput").ap()

    es = ExitStack()
    with es:
        def sb(n, s, d):
            return es.enter_context(nc.sbuf_tensor(n, list(s), d))

        X = sb("X", [128, NT, D], F32)
        XT = sb("XT", [128, 8, S], BF16)
        YTb = sb("YTb", [128, 4, S], BF16)
        LNP = sb("LNP", [128, 2048], F32)
        BIG = sb("BIG", [128, 37184], BF16)
        ident = sb("ident", [128, 128], BF16)
        trim = sb("trim", [128, 128], BF16)
        HB = sb("HB", [128, 2, D], BF16)
        PT = sb("PT", [128, 4, 512], BF16)
        BT = sb("BT", [128, 4, 256], F32)
        SM = sb("SM", [128, 512], F32)
        EPSB = sb("EPSB", [128, 1], F32)
        PS = [es.enter_context(nc.psum_tensor("ps%d" % i, [128, 512], F32)) for i in range(8)]

        P = Prog(nc)
        tX = [T("X%d" % t) for t in range(NT)]
        tXT = [T("XT%d" % t) for t in range(NT)]
        tYTb = [T("YTb%d" % t) for t in range(NT)]
        tPS = [T("ps%d" % i) for i in range(8)]
        tW = [T("w%d" % i) for i in range(6)]
        tHB = [T("hb0"), T("hb1")]
        tPT = [T("pt%d" % i) for i in range(4)]
        tBT = [T("bt%d" % i) for i in range(4)]
        tid = T("ident")
        ttr = T("trim")
        tLNP = T("lnp")

        A0 = 3 * 4096
        E0 = 6 * 4096

        class Region:
            def __init__(self, base, size):
                self.base, self.size, self.cur = base, size, 0

            def reset(self):
                self.cur = 0

            def take(self, nbytes, dt=BF16):
                n = (nbytes + 3) // 4 * 2
                assert self.cur + n <= self.size, (self.cur, n, self.size)
                ap = BIG[:, self.base + self.cur:self.base + self.cur + n]
                self.cur += n
                if dt == F32:
                    ap = ap.bitcast(F32)
                return ap

        ARENA = Region(A0, 37184 - A0)
        EXTRA = Region(E0, 37184 - E0)

        def wslot(s, c, f=None):
            f = f or 4096 // c
            return BIG[:, s * 4096:s * 4096 + c * f].rearrange("p (c f) -> p c f", c=c)

        def load_w(s, dram_ap, c):
            P.dma("pool", wslot(s, c, dram_ap.shape[1]), dram_ap.rearrange("(c p) f -> p c f", p=128), writes=[tW[s]])

        P.op("dve", lambda e: e.memset(ident[:], 1.0), writes=[tid])
        P.op("dve", lambda e: e.memset(EPSB[:], EPS), writes=[tid])
        P.op("pool", lambda e: e.affine_select(out=ident[:], in_=ident[:], pattern=[[-1, 128]],
                                               compare_op=ALU.is_equal, fill=0.0, base=0, channel_multiplier=1),
             reads=[tid], writes=[tid])
        P.op("dve", lambda e: e.memset(trim[:], 1.0), writes=[ttr])
        P.op("pool", lambda e: e.affine_select(out=trim[:], in_=trim[:], pattern=[[1, 128]],
                                               compare_op=ALU.is_ge, fill=0.0, base=0, channel_multiplier=-1),
             reads=[ttr], writes=[ttr])

        cp_flip = [0]

        def evac(out, in_, reads, writes, scale=None):
            cp_flip[0] ^= 1
            if scale is not None:
                P.op("act", lambda e: e.mul(out=out, in_=in_, mul=scale), reads=reads, writes=writes)
            elif cp_flip[0]:
                P.op("act", lambda e: e.copy(out=out, in_=in_), reads=reads, writes=writes)
            else:
                P.op("dve", lambda e: e.tensor_copy(out=out, in_=in_), reads=reads, writes=writes)

        def transposes(src_fn, nblk, src_reads, bank, dst, dst_writes):
            pv = PS[bank][:].bitcast(BF16)
            for k in range(nblk):
                P.op("pe", lambda e, k=k: e.transpose(out=pv[:, k * 128:(k + 1) * 128], in_=src_fn(k), identity=ident[:]),
                     reads=list(src_reads) + [tid], writes=[tPS[bank]])
            evac(dst, pv[:, 0:nblk * 128].rearrange("p (c f) -> p c f", c=nblk), [tPS[bank]], dst_writes)

        def x_to_xt(t, bank, hb=None):
            if hb is None:
                hb = t % 2
            P.op("act", lambda e: e.copy(out=HB[:, hb, :], in_=X[:, t, :]), reads=[tX[t]], writes=[tHB[hb]])
            transposes(lambda k: HB[:, hb, k * 128:(k + 1) * 128], 8, [tHB[hb]], bank,
                       XT[:, :, t * 128:(t + 1) * 128], [tXT[t]])

        tSt = [T("st%d" % t) for t in range(NT)]

        def ln_group(tiles, gi):
            st = SM[:, 0:192].rearrange("p (t s) -> p t s", t=16)
            mv = SM[:, 192:256].rearrange("p (t s) -> p t s", t=16)
            t0, t1 = tiles[0], tiles[-1] + 1
            ts_ = [tSt[t] for t in tiles]
            for t in tiles:
                P.op("dve", lambda e, t=t: e.bn_stats(out=st[:, t, 0:6], in_=X[:, t, 0:512]), reads=[tX[t]], writes=[tSt[t]])
                P.op("dve", lambda e, t=t: e.bn_stats(out=st[:, t, 6:12], in_=X[:, t, 512:1024]), reads=[tX[t]], writes=[tSt[t]])
            for t in tiles:
                P.op("dve", lambda e, t=t: e.bn_aggr(out=mv[:, t, 0:2], in_=st[:, t, :]), reads=[tSt[t]], writes=[tSt[t]])
            P.op("act", lambda e: e.activation(out=mv[:, t0:t1, 2], in_=mv[:, t0:t1, 1], func=AF.Sqrt, bias=EPSB[:, 0:1]),
                 reads=ts_ + [tid], writes=ts_)
            P.op("dve", lambda e: e.reciprocal(out=mv[:, t0:t1, 2], in_=mv[:, t0:t1, 2]), reads=ts_, writes=ts_)
            P.op("dve", lambda e: e.scalar_tensor_tensor(out=mv[:, t0:t1, 3], in0=mv[:, t0:t1, 0], scalar=-1.0,
                                                         in1=mv[:, t0:t1, 2], op0=ALU.mult, op1=ALU.mult), reads=ts_, writes=ts_)
            for t in tiles:
                P.op("act", lambda e, t=t: e.activation(out=X[:, t, :], in_=X[:, t, :], func=AF.Identity,
                                                        scale=mv[:, t, 2:3], bias=mv[:, t, 3:4]), reads=[tX[t], tSt[t]], writes=[tX[t]])
            for t in tiles:
                P.op("dve", lambda e, t=t: e.tensor_tensor(out=X[:, t, :], in0=X[:, t, :], in1=LNP[:, gi * 1024:(gi + 1) * 1024],
                                                           op=ALU.mult), reads=[tX[t], tLNP], writes=[tX[t]])
                P.op("pool", lambda e, t=t: e.tensor_tensor(out=X[:, t, :], in0=X[:, t, :],
                                                            in1=LNP[:, (gi + 1) * 1024:(gi + 2) * 1024], op=ALU.add),
                     reads=[tX[t], tLNP], writes=[tX[t]])

        def load_lnp(l, which):
            P.dma("sp", LNP[:].rearrange("p (a f) -> p a f", a=2),
                  Ld[l]["lnp"][2 * which:2 * which + 2, :].unsqueeze(0).broadcast_to([128, 2, D]), writes=[tLNP])

        for t in range(NT):
            P.dma("sp", X[:, t, :], x_d[t * 128:(t + 1) * 128, :], writes=[tX[t]])
        for t in range(NT):
            x_to_xt(t, t % 2)

        def proj_feat(ws, dst, dst_t, nchunk, banks):
            W = wslot(ws, 8)
            k = 0
            for ch in range(nchunk):
                for g in range(4):
                    b = banks[k % len(banks)]
                    k += 1
                    for c in range(8):
                        P.op("pe", lambda e, c=c, ch=ch, g=g, b=b: e.matmul(
                            PS[b][:], lhsT=W[:, c, ch * 128:(ch + 1) * 128], rhs=XT[:, c, g * 512:(g + 1) * 512],
                            start=(c == 0), stop=(c == 7)),
                            reads=[tW[ws]] + tXT[4 * g:4 * g + 4], writes=[tPS[b]])
                    evac(dst[:, ch, g * 512:(g + 1) * 512], PS[b][:], [tPS[b]], [dst_t[ch]])

        def proj_tok(ws, t, b, ncols=512):
            W = wslot(ws, 8)
            for c in range(8):
                P.op("pe", lambda e, c=c: e.matmul(PS[b][:, 0:ncols], lhsT=XT[:, c, t * 128:(t + 1) * 128],
                                                   rhs=W[:, c, 0:ncols], start=(c == 0), stop=(c == 7)),
                     reads=[tW[ws], tXT[t]], writes=[tPS[b]])

        PT8 = PT[:].rearrange("p a f -> p (a f)").rearrange("p (a f) -> p a f", a=8)
        tPT8 = [T("pt8_%d" % i) for i in range(8)]

        def attention(nunits, unit_part, unit_chunk, unit_v, QT, KT, tQ, tK, V, tV, dv1, G, bias_fn, near_fn,
                      epilogue, after_group, acc_map, diag_mask, look=1):
            ngroups = NT // G

            def unit_body(g, u, stream):
                nbuf = look + 1
                hp = unit_part(u)
                hc = unit_chunk(u)
                if look == 1:
                    sbk = [2 * stream, 2 * stream + 1]
                    S_ap = lambda j: PS[sbk[j % 2]][:]
                    S_t = lambda j: tPS[sbk[j % 2]]
                    PT_ap = lambda j: PT[:, 2 * stream + j % 2, :]
                    PT_t = lambda j: tPT[2 * stream + j % 2]
                else:
                    S_ap = lambda j: PS[3 * stream + j % 3][:]
                    S_t = lambda j: tPS[3 * stream + j % 3]
                    PT_ap = lambda j: PT8[:, 3 * stream + j % 3, :]
                    PT_t = lambda j: tPT8[3 * stream + j % 3]

                def acc(i, u=u):
                    b, off = acc_map(u, i)
                    return PS[b][:, off:off + dv1]

                def acc_t(i, u=u):
                    return tPS[acc_map(u, i)[0]]

                jmax = G * g + G - 1
                started = set()
                info = {}

                def score(j):
                    ilo = max(j, G * g)
                    nb = G * g + G - ilo
                    sap, st_, pap, pt_ = S_ap(j), S_t(j), PT_ap(j), PT_t(j)
                    P.op("pe", lambda e, j=j, ilo=ilo, nb=nb, sap=sap: e.matmul(
                        sap[:, 0:nb * 128], lhsT=KT[hp:hp + 64, hc, j * 128:(j + 1) * 128],
                        rhs=QT[hp:hp + 64, hc, ilo * 128:(ilo + nb) * 128], start=True, stop=True),
                        reads=[tK[hc], tQ[hc]], writes=[st_])
                    done = near_fn(u, g, j, ilo, nb, sap, st_, pap, pt_, stream)
                    if done < nb:
                        bias, breads = bias_fn(u, g, j)
                        P.op("act", lambda e, sap=sap, pap=pap, done=done, nb=nb, bias=bias: e.activation(
                            out=pap[:, done * 128:nb * 128], in_=sap[:, done * 128:nb * 128],
                            func=AF.Exp, scale=0.125, bias=bias),
                            reads=[st_] + breads, writes=[pt_])
                    if diag_mask and ilo == j:
                        P.op("dve", lambda e, pap=pap: e.tensor_tensor(out=pap[:, 0:128], in0=pap[:, 0:128],
                                                                       in1=trim[:], op=ALU.mult),
                             reads=[pt_, ttr], writes=[pt_])
                    info[j] = (j, ilo, nb, pap, pt_)

                for j in range(min(look, jmax + 1)):
                    score(j)
                for j in range(jmax + 1):
                    if j + look <= jmax:
                        score(j + look)
                    emit_pv(info.pop(j), u, g, acc, acc_t, V, tV, unit_v, dv1, G, started, acc_map)
                epilogue(u, g, acc, acc_t)

            for g in range(ngroups):
                for u0 in range(0, nunits, 2):
                    P.begin_capture()
                    unit_body(g, u0, 0)
                    la = P.end_capture()
                    P.begin_capture()
                    unit_body(g, u0 + 1, 1)
                    lb = P.end_capture()
                    P.replay([la, lb])
                after_group(g)

        def emit_pv(pend, u, g, acc, acc_t, V, tV, unit_v, dv1, G, started, acc_map):
            j, ilo, nb, pap, pt_ = pend
            for k in range(nb):
                i = ilo + k
                ii = i - G * g
                bank = acc_map(u, ii)[0]
                st = False
                if j == 0 and bank not in started:
                    started.add(bank)
                    st = True
                P.op("pe", lambda e, k=k, ii=ii, j=j, i=i, pap=pap, st=st: e.matmul(
                    acc(ii), lhsT=pap[:, k * 128:(k + 1) * 128], rhs=V[:, j, unit_v(u), 0:dv1],
                    start=st, stop=(j == i), skip_group_check=True),
                    reads=[pt_, tV[j]], writes=[acc_t(ii)])

        def ffn_block(l, last):
            d = Ld[l]
            if os.environ.get("KBAR", "0") == "1":
                P.barrier()
            EXTRA.reset()
            tE = T("extra")
            Wr32 = EXTRA.take(8 * 16 * 4, F32).rearrange("p (c f) -> p c f", c=8)
            Wr = EXTRA.take(8 * 16 * 2).rearrange("p (c f) -> p c f", c=8)
            tWr = T("wr")
            P.dma("sp", Wr32, router_d.rearrange("(c p) f -> p c f", p=128), writes=[tWr])
            P.op("dve", lambda e: e.tensor_copy(out=Wr, in_=Wr32), reads=[tWr], writes=[tWr])
            def load_expert(e_, base):
                load_w(base + 0, d["wg"][e_], 8)
                load_w(base + 1, d["wu"][e_], 8)
                load_w(base + 2, d["wd"][e_], 4)
            load_expert(0, 0)
            load_expert(1, 3)
            for t in range(NT):
                for c in range(8):
                    P.op("pe", lambda e, t=t, c=c: e.matmul(PS[7][:, t * 16:(t + 1) * 16], lhsT=XT[:, c, t * 128:(t + 1) * 128],
                                                         rhs=Wr[:, c, :], start=(c == 0), stop=(c == 7)),
                         reads=[tXT[t], tWr], writes=[tPS[7]])
            tG = T("gate")
            def f32t(n):
                return EXTRA.take(n * 4, F32)
            L = f32t(256); E_ = f32t(256); E2 = f32t(256); EQ = f32t(256); SEL = f32t(256); GATE = f32t(256)
            mx = f32t(16); m1 = f32t(64); m2 = f32t(64); gs = f32t(64); gm = f32t(16); gsel = f32t(64); den = f32t(16)
            L3 = L.rearrange("p (t e) -> p t e", t=16)
            def v4(ap):
                return ap.rearrange("p (t e) -> p t e", e=4)
            P.op("act", lambda e: e.copy(out=L, in_=PS[7][:, 0:256]), reads=[tPS[7]], writes=[tG])
            P.op("dve", lambda e: e.tensor_reduce(out=mx, in_=L3, axis=AX.X, op=ALU.max), reads=[tG], writes=[tG])
            P.op("dve", lambda e: e.tensor_tensor(out=L3, in0=L3, in1=mx.unsqueeze(2).broadcast_to([128, 16, 16]),
                                                  op=ALU.subtract), reads=[tG], writes=[tG])
            P.op("act", lambda e: e.activation(out=E_, in_=L, func=AF.Exp), reads=[tG], writes=[tG])
            P.op("dve", lambda e: e.tensor_reduce(out=m1, in_=v4(E_), axis=AX.X, op=ALU.max), reads=[tG], writes=[tG])
            P.op("dve", lambda e: e.tensor_tensor(out=v4(EQ), in0=v4(E_), in1=m1.unsqueeze(2).broadcast_to([128, 64, 4]),
                                                  op=ALU.is_equal), reads=[tG], writes=[tG])
            P.op("dve", lambda e: e.scalar_tensor_tensor(out=E2, in0=EQ, scalar=-4.0, in1=E_, op0=ALU.mult, op1=ALU.add),
                 reads=[tG], writes=[tG])
            P.op("dve", lambda e: e.tensor_reduce(out=m2, in_=v4(E2), axis=AX.X, op=ALU.max), reads=[tG], writes=[tG])
            P.op("dve", lambda e: e.tensor_tensor(out=gs, in0=m1, in1=m2, op=ALU.add), reads=[tG], writes=[tG])
            P.op("dve", lambda e: e.tensor_reduce(out=gm, in_=v4(gs), axis=AX.X, op=ALU.max), reads=[tG], writes=[tG])
            P.op("dve", lambda e: e.tensor_tensor(out=v4(gsel), in0=v4(gs), in1=gm.unsqueeze(2).broadcast_to([128, 16, 4]),
                                                  op=ALU.is_equal), reads=[tG], writes=[tG])
            P.op("dve", lambda e: e.tensor_tensor(out=v4(SEL), in0=v4(E_), in1=m2.unsqueeze(2).broadcast_to([128, 64, 4]),
                                                  op=ALU.is_ge), reads=[tG], writes=[tG])
            P.op("dve", lambda e: e.tensor_tensor(out=v4(SEL), in0=v4(SEL), in1=gsel.unsqueeze(2).broadcast_to([128, 64, 4]),
                                                  op=ALU.mult), reads=[tG], writes=[tG])
            P.op("dve", lambda e: e.tensor_tensor(out=E2, in0=E_, in1=SEL, op=ALU.mult), reads=[tG], writes=[tG])
            P.op("dve", lambda e: e.tensor_reduce(out=den, in_=E2.rearrange("p (t e) -> p t e", t=16), axis=AX.X, op=ALU.add),
                 reads=[tG], writes=[tG])
            P.op("dve", lambda e: e.reciprocal(out=den, in_=den), reads=[tG], writes=[tG])
            P.op("dve", lambda e: e.tensor_tensor(out=GATE.rearrange("p (t e) -> p t e", t=16),
                                                  in0=E2.rearrange("p (t e) -> p t e", t=16),
                                                  in1=den.unsqueeze(2).broadcast_to([128, 16, 16]), op=ALU.mult),
                 reads=[tG], writes=[tG])
            for t in range(NT):
                P.op("act", lambda e, t=t: e.mul(out=X[:, t, :], in_=X[:, t, :], mul=ALPHA), reads=[tX[t]], writes=[tX[t]])
            AT = [EXTRA.take(4 * 512 * 2).rearrange("p (c f) -> p c f", c=4) for _ in range(2)]
            tAT = [T("at0"), T("at1")]
            SG = [EXTRA.take(512 * 2) for _ in range(2)]
            tSG = [T("sg0"), T("sg1")]
            kc = {"gu": 0, "y": 0}

            def GU(e_, g):
                base = (e_ % 2) * 3
                Wg = wslot(base, 8); Wu = wslot(base + 1, 8)
                ab = g % 2
                for fc in range(4):
                    bg = (kc["gu"] % 2) * 2
                    bu = bg + 1
                    si = kc["gu"] % 2
                    kc["gu"] += 1
                    for c in range(8):
                        P.op("pe", lambda e, c=c, fc=fc, g=g, bg=bg, Wg=Wg: e.matmul(
                            PS[bg][:], lhsT=Wg[:, c, fc * 128:(fc + 1) * 128], rhs=XT[:, c, g * 512:(g + 1) * 512],
                            start=(c == 0), stop=(c == 7)), reads=[tW[base]] + tXT[4 * g:4 * g + 4], writes=[tPS[bg]])
                    for c in range(8):
                        P.op("pe", lambda e, c=c, fc=fc, g=g, bu=bu, Wu=Wu: e.matmul(
                            PS[bu][:], lhsT=Wu[:, c, fc * 128:(fc + 1) * 128], rhs=XT[:, c, g * 512:(g + 1) * 512],
                            start=(c == 0), stop=(c == 7)), reads=[tW[base + 1]] + tXT[4 * g:4 * g + 4], writes=[tPS[bu]])
                    P.op("act", lambda e, bg=bg, si=si: e.activation(out=SG[si], in_=PS[bg][:], func=AF.Silu),
                         reads=[tPS[bg]], writes=[tSG[si]])
                    P.op("dve", lambda e, bu=bu, si=si, ab=ab, fc=fc: e.tensor_tensor(
                        out=AT[ab][:, fc, :], in0=SG[si], in1=PS[bu][:], op=ALU.mult),
                        reads=[tSG[si], tPS[bu]], writes=[tAT[ab]])

            def DOWN(e_, g):
                base = (e_ % 2) * 3
                Wd = wslot(base + 2, 4)
                ab = g % 2
                for tt in range(4):
                    t = 4 * g + tt
                    for n in range(2):
                        by = 4 + (kc["y"] % 4)
                        kc["y"] += 1
                        for fc in range(4):
                            P.op("pe", lambda e, fc=fc, tt=tt, n=n, by=by, ab=ab, Wd=Wd: e.matmul(
                                PS[by][:], lhsT=AT[ab][:, fc, tt * 128:(tt + 1) * 128], rhs=Wd[:, fc, n * 512:(n + 1) * 512],
                                start=(fc == 0), stop=(fc == 3)), reads=[tAT[ab], tW[base + 2]], writes=[tPS[by]])
                        P.op("dve", lambda e, t=t, n=n, by=by, e_=e_: e.scalar_tensor_tensor(
                            out=X[:, t, n * 512:(n + 1) * 512], in0=PS[by][:], scalar=GATE[:, t * 16 + e_:t * 16 + e_ + 1],
                            in1=X[:, t, n * 512:(n + 1) * 512], op0=ALU.mult, op1=ALU.add),
                            reads=[tPS[by], tG, tX[t]], writes=[tX[t]])

            steps = [(e_, g) for e_ in range(16) for g in range(4)]
            GU(*steps[0])
            for i_, (e_, g) in enumerate(steps):
                if i_ + 1 < len(steps):
                    GU(*steps[i_ + 1])
                DOWN(e_, g)
                if g == 3 and e_ + 2 < 16:
                    load_expert(e_ + 2, (e_ % 2) * 3)
            load_lnp(l, 1)
            load_w(0, d["pgate"][:, 0:512], 8)
            load_w(1, d["pgate"][:, 512:1024], 8)
            load_w(2, d["pproj"], 2)
            Wp0 = wslot(0, 8); Wp1 = wslot(1, 8); Wpp = wslot(2, 2, 1024)
            P.barrier()
            EXTRA.reset()
            pT = EXTRA.take(2 * S * 2).rearrange("p (c f) -> p c f", c=2)
            tpT = [T("pT%d" % t) for t in range(NT)]
            P32 = [EXTRA.take(256 * 4, F32) for _ in range(4)]
            Pb = [EXTRA.take(256 * 2) for _ in range(4)]
            tP32 = [T("p32_%d" % i) for i in range(4)]
            tPb = [T("pb_%d" % i) for i in range(4)]
            SGF = [EXTRA.take(512 * 4, F32) for _ in range(4)]
            tSGF = [T("sgf%d" % i) for i in range(4)]

            def pprep():
                for t in range(NT):
                    k = t % 4
                    P.dma("sp", P32[k], d["p"][t * 128:(t + 1) * 128, :], writes=[tP32[k]])
                    P.op("act", lambda e, k=k: e.copy(out=Pb[k], in_=P32[k]), reads=[tP32[k]], writes=[tPb[k]])
                    transposes(lambda kk, k=k: Pb[k][:, kk * 128:(kk + 1) * 128], 2, [tPb[k]], 6,
                               pT[:, :, t * 128:(t + 1) * 128], [tpT[t]])
            kk = [0]

            def A(grp):
                ln_group(grp, 0)
                for t in grp:
                    x_to_xt(t, 7, 0)

            def B(grp):
                for t in grp:
                    for n in range(2):
                        ba = (kk[0] % 2) * 2
                        bb = ba + 1
                        si = kk[0] % 4
                        kk[0] += 1
                        Wp = Wp0 if n == 0 else Wp1
                        for c in range(8):
                            P.op("pe", lambda e, c=c, t=t, ba=ba, Wp=Wp: e.matmul(
                                PS[ba][:], lhsT=XT[:, c, t * 128:(t + 1) * 128], rhs=Wp[:, c, :], start=(c == 0), stop=(c == 7)),
                                reads=[tXT[t], tW[n]], writes=[tPS[ba]])
                        for c in range(2):
                            P.op("pe", lambda e, c=c, t=t, bb=bb, n=n: e.matmul(
                                PS[bb][:], lhsT=pT[:, c, t * 128:(t + 1) * 128], rhs=Wpp[:, c, n * 512:(n + 1) * 512],
                                start=(c == 0), stop=(c == 1)), reads=[tpT[t], tW[2]], writes=[tPS[bb]])
                        P.op("act", lambda e, ba=ba, si=si: e.activation(out=SGF[si], in_=PS[ba][:], func=AF.Sigmoid),
                             reads=[tPS[ba]], writes=[tSGF[si]])
                        P.op("dve", lambda e, bb=bb, si=si: e.tensor_tensor(out=SGF[si], in0=SGF[si], in1=PS[bb][:], op=ALU.mult),
                             reads=[tSGF[si], tPS[bb]], writes=[tSGF[si]])
                        P.op("pool", lambda e, t=t, n=n, si=si: e.tensor_tensor(
                            out=X[:, t, n * 512:(n + 1) * 512], in0=X[:, t, n * 512:(n + 1) * 512], in1=SGF[si], op=ALU.add),
                            reads=[tSGF[si], tX[t]], writes=[tX[t]])
                    if last:
                        P.dma("sp", y_d[t * 128:(t + 1) * 128, :], X[:, t, :], reads=[tX[t]])
                    else:
                        x_to_xt(t, 6, 1)

            NG = int(os.environ.get("KNG", "2"))
            GS_ = NT // NG
            grps = [list(range(GS_ * g, GS_ * g + GS_)) for g in range(NG)]
            P.begin_capture()
            A(grps[0])
            la = P.end_capture()
            P.begin_capture()
            pprep()
            lb = P.end_capture()
            P.replay([la, lb])
            for g in range(1, NG):
                P.begin_capture()
                A(grps[g])
                la = P.end_capture()
                P.begin_capture()
                B(grps[g - 1])
                lb = P.end_capture()
                P.replay([la, lb])
            B(grps[NG - 1])

        def out_proj_ln1(l, ychunk):
            d = Ld[l]
            load_w(0, d["w_out"][:, 0:512], 8)
            load_w(1, d["w_out"][:, 512:1024], 8)
            P.barrier()
            load_lnp(l, 0)
            Wo = [wslot(0, 8), wslot(1, 8)]
            kk = [0]

            def OP(grp):
                for t in grp:
                    for n in range(2):
                        b = kk[0] % 4
                        kk[0] += 1
                        for c in range(8):
                            src, tsrc = ychunk(c)
                            P.op("pe", lambda e, c=c, t=t, n=n, b=b, src=src: e.matmul(
                                PS[b][:], lhsT=src[:, t * 128:(t + 1) * 128], rhs=Wo[n][:, c, :], start=(c == 0), stop=(c == 7)),
                                reads=[tsrc[t], tW[n]], writes=[tPS[b]])
                        P.op("dve", lambda e, t=t, n=n, b=b: e.scalar_tensor_tensor(
                            out=X[:, t, n * 512:(n + 1) * 512], in0=X[:, t, n * 512:(n + 1) * 512], scalar=ALPHA, in1=PS[b][:],
                            op0=ALU.mult, op1=ALU.add), reads=[tPS[b], tX[t]], writes=[tX[t]])

            def LNX(grp):
                ln_group(grp, 0)
                for t in grp:
                    x_to_xt(t, 4 + t % 2)

            NG = int(os.environ.get("KNG", "2"))
            GS_ = NT // NG
            grps = [list(range(GS_ * g, GS_ * g + GS_)) for g in range(NG)]
            OP(grps[0])
            for g in range(1, NG):
                P.begin_capture()
                OP(grps[g])
                la = P.end_capture()
                P.begin_capture()
                LNX(grps[g - 1])
                lb = P.end_capture()
                P.replay([la, lb])
            LNX(grps[NG - 1])

        def layer_odd(l):
            d = Ld[l]
            P.barrier()
            ARENA.reset()
            QT = ARENA.take(4 * S * 2).rearrange("p (c f) -> p c f", c=4)
            KT = ARENA.take(4 * S * 2).rearrange("p (c f) -> p c f", c=4)
            V = ARENA.take(16 * 8 * 65 * 2).rearrange("p (t h e) -> p t h e", t=16, h=8)
            tQ = [T("q%d" % i) for i in range(4)]
            tK = [T("k%d" % i) for i in range(4)]
            tV = [T("v%d" % i) for i in range(NT)]
            CUM = LNP[:, 0:256]
            OFF = LNP[:, 256:528]
            TOT = LNP[:, 528:784]
            Z = LNP[:, 784:1040]
            BFB = LNP[:, 1040:1056]
            WF32 = LNP[:, 1056:1184].rearrange("p (c f) -> p c f", c=8)
            UT = LNP[:, 1184:1312]
            ONES = LNP[:, 1312:1440]
            YB = LNP[:, 1440:1952].bitcast(BF16).rearrange("p (a f) -> p a f", a=2)
            WFb = SM[:, 256:320].bitcast(BF16).rearrange("p (c f) -> p c f", c=8)
            tF = T("forget")
            P.dma("sp", WF32, d["w_in"][:, 3072:3088].rearrange("(c p) f -> p c f", p=128), writes=[tF, tLNP])
            P.dma("sp", BFB, d["bf"].broadcast_to([128, 16]), writes=[tF])
            P.op("dve", lambda e: e.tensor_copy(out=WFb, in_=WF32), reads=[tF], writes=[tF])
            P.op("dve", lambda e: e.memset(ONES, 1.0), writes=[tF])
            P.op("dve", lambda e: e.memset(UT, 1.0), writes=[tF])
            P.op("pool", lambda e: e.affine_select(out=UT, in_=UT, pattern=[[1, 128]], compare_op=ALU.is_ge, fill=0.0,
                                                   base=0, channel_multiplier=-1), reads=[tF], writes=[tF])
            for t in range(NT):
                for c in range(8):
                    P.op("pe", lambda e, t=t, c=c: e.matmul(PS[7][:, t * 16:(t + 1) * 16], lhsT=XT[:, c, t * 128:(t + 1) * 128],
                                                         rhs=WFb[:, c, :], start=(c == 0), stop=(c == 7)),
                         reads=[tXT[t], tF], writes=[tPS[7]])
            Z3 = Z.rearrange("p (t h) -> p t h", t=16)
            P.op("dve", lambda e: e.tensor_tensor(out=Z3, in0=PS[7][:, 0:256].rearrange("p (t h) -> p t h", t=16),
                                                  in1=BFB.unsqueeze(1).broadcast_to([128, 16, 16]), op=ALU.add),
                 reads=[tPS[7], tF], writes=[tF])
            P.op("act", lambda e: e.activation(out=Z, in_=Z, func=AF.Exp, scale=-1.0), reads=[tF], writes=[tF])
            P.op("act", lambda e: e.activation(out=Z, in_=Z, func=AF.Ln, bias=1.0), reads=[tF], writes=[tF])
            P.op("pe", lambda e: e.matmul(PS[6][:, 0:256], lhsT=UT, rhs=Z, start=True, stop=True), reads=[tF], writes=[tPS[6]])
            P.op("pe", lambda e: e.matmul(PS[7][:, 256:512], lhsT=ONES, rhs=Z, start=True, stop=True), reads=[tF], writes=[tPS[7]])
            P.op("dve", lambda e: e.tensor_copy(out=TOT, in_=PS[7][:, 256:512]), reads=[tPS[7]], writes=[tF])
            OFF3 = OFF.rearrange("p (t h) -> p t h", t=17)
            TOT3 = TOT.rearrange("p (t h) -> p t h", t=16)
            P.op("dve", lambda e: e.memset(OFF3[:, 0, :], 0.0), writes=[tF])
            for t in range(NT):
                P.op("dve", lambda e, t=t: e.tensor_tensor(out=OFF3[:, t + 1, :], in0=OFF3[:, t, :], in1=TOT3[:, t, :], op=ALU.add),
                     reads=[tF], writes=[tF])
            P.op("dve", lambda e: e.tensor_tensor(out=CUM, in0=PS[6][:, 0:256], in1=OFF[:, 0:256], op=ALU.add),
                 reads=[tPS[6], tF], writes=[tF])
            CUM3 = CUM.rearrange("p (t h) -> p t h", t=16)
            BIAS = SM[:, 320:384]
            tB = [T("b%d" % i) for i in range(4)]

            for half in range(2):
                if half == 1:
                    P.barrier()
                load_w(0, d["w_in"][:, half * 512:half * 512 + 512], 8)
                load_w(1, d["w_in"][:, 1024 + half * 512:1024 + half * 512 + 512], 8)
                load_w(2, d["w_in"][:, 2048 + half * 512:2048 + half * 512 + 512], 8)
                proj_feat(0, QT, tQ, 4, [0, 1, 2, 3])
                proj_feat(1, KT, tK, 4, [0, 1, 2, 3])
                P.op("dve", lambda e: e.memset(V[:, :, :, 64:65], 1.0), writes=tV)
                for t in range(NT):
                    b = 4 + t % 4
                    proj_tok(2, t, b)
                    evac(V[:, t, :, 0:64], PS[b][:].rearrange("p (h e) -> p h e", h=8), [tPS[b]], [tV[t]])

                bk = [0, 0]
                cur_bias = {}

                def bias_fn(u, g, j):
                    return cur_bias[u][0][:, j:j + 1], [cur_bias[u][1]]

                def near_fn(u, g, j, ilo, nb, sap, st_, pap, pt_, stream):
                    if j == 0:
                        k = 2 * stream + bk[stream] % 2
                        bk[stream] += 1
                        hg = half * 8 + u
                        ap = BIAS[:, k * 16:(k + 1) * 16]
                        P.op("dve", lambda e, ap=ap, hg=hg, g=g: e.tensor_scalar(
                            out=ap, in0=CUM3[:, :, hg], scalar1=OFF3[:, 2 * g + 1, hg:hg + 1], scalar2=None,
                            op0=ALU.subtract), reads=[tF], writes=[tB[k]])
                        cur_bias[u] = (ap, tB[k])
                    return 0

                def post_exp_mask(j, ilo, pt):
                    pass

                def epilogue(u, g, acc, acc_t):
                    k = u % 2
                    rs = SM[:, 384 + 2 * (u % 8):384 + 2 * (u % 8) + 2]
                    trs = tRS[u % 8]
                    for ii in range(2):
                        P.op("dve", lambda e, ii=ii, rs=rs: e.reciprocal(out=rs[:, ii:ii + 1], in_=acc(ii)[:, 64:65]),
                             reads=[acc_t(ii)], writes=[trs])
                        P.op("dve", lambda e, ii=ii, rs=rs, u=u: e.tensor_scalar(
                            out=YB[:, ii, u * 64:(u + 1) * 64], in0=acc(ii)[:, 0:64], scalar1=rs[:, ii:ii + 1], scalar2=None,
                            op0=ALU.mult), reads=[acc_t(ii), trs], writes=[tYB[ii]])

                tRS = [T("rs%d" % i) for i in range(8)]
                tYB = [T("yb0"), T("yb1")]

                def after_group(g):
                    for ii in range(2):
                        t = 2 * g + ii
                        if half == 0:
                            transposes(lambda k, ii=ii: YB[:, ii, k * 128:(k + 1) * 128], 4, [tYB[ii]], 3 * ii,
                                       YTb[:, :, t * 128:(t + 1) * 128], [tYTb[t]])
                        else:
                            transposes(lambda k, ii=ii: YB[:, ii, k * 128:(k + 1) * 128], 4, [tYB[ii]], 3 * ii,
                                       XT[:, 0:4, t * 128:(t + 1) * 128], [tXT[t]])

                attention(8, lambda u: 64 * (u % 2), lambda u: u // 2, lambda u: u, QT, KT, tQ, tK, V, tV, 65, 2,
                          bias_fn, near_fn, epilogue, after_group, lambda u, i: (6 + u % 2, i * 65), True, look=2)

            def ychunk(c):
                if c < 4:
                    return YTb[:, c, :], tYTb
                return XT[:, c - 4, :], tXT
            out_proj_ln1(l, ychunk)


        def layer_even(l):
            d = Ld[l]
            lam_init = 0.8 - 0.6 * math.exp(-0.3 * l)
            P.barrier()
            ARENA.reset()
            tC = T("consts")
            CT = ARENA.take(NCONST * 4, F32)
            P.dma("sp", CT, consts_d, writes=[tC])
            COS = CT[:, C_COS:C_COS + 512].rearrange("p (t f) -> p t f", t=16)
            SIN = CT[:, C_SIN:C_SIN + 512].rearrange("p (t f) -> p t f", t=16)
            DM = CT[:, C_DM:C_DM + 512].rearrange("p (h c) -> p h c", h=4)
            QD = CT[:, C_QD:C_QD + 4]
            KD = CT[:, C_KD:C_KD + 4]
            RG = ARENA.take(128 * 4, F32)
            P.dma("sp", RG, d["rg"].broadcast_to([128, 128]), writes=[tC])
            load_w(0, d["w_in"][:, 1536:2048], 8)
            load_w(1, d["w_in"][:, 2048:2560], 8)
            load_w(2, d["w_in"][:, 2560:3072], 8)
            ST32 = ARENA.take(512 * 4, F32).rearrange("p (b e) -> p b e", b=4)
            STB = ARENA.take(512 * 2).rearrange("p (b e) -> p b e", b=4)
            KZ = [ARENA.take(512 * 2).rearrange("p (b e) -> p b e", b=4) for _ in range(2)]
            tKZ = [T("kz0"), T("kz1")]
            for k_ in range(2):
                P.op("dve", lambda e, k_=k_: e.memset(KZ[k_], 0.0), writes=[tKZ[k_]])
            tST = T("st32")
            tSTB = T("stb")
            P.op("dve", lambda e: e.memset(ST32, 0.0), writes=[tST])

            def two(nbytes, dt=BF16):
                return [ARENA.take(nbytes, dt) for _ in range(2)]
            T1a = two(1024, F32); T2a = two(1024, F32); T1b = two(1024, F32); T2b = two(1024, F32)
            QKR = two(2048, F32); QKS = two(1024); KRD = two(512); QKT = two(1024); VB = two(1024)
            GS = two(2048, F32); SCT = two(1024); OSB = two(2048, F32); YN = two(2048, F32); YBt = two(1024)
            STT = two(24 * 4, F32); MV = two(16 * 4, F32)
            names = "t1a t2a t1b t2b qkr qks krd qrt krt vb gs sct osb yn ybt stt".split()
            tt_ = {n: [T(n + "0"), T(n + "1")] for n in names}
            def ret_front(i):
                k = i % 2
                t = i
                proj_tok(0, t, 0)
                qk = PS[0][:].rearrange("p (h a f) -> p h a f", h=8, a=2)
                qkr = QKR[k].rearrange("p (h a f) -> p h a f", h=8, a=2)
                cosb = COS[:, t, :].unsqueeze(1).broadcast_to([128, 8, 32])
                sinb = SIN[:, t, :].unsqueeze(1).broadcast_to([128, 8, 32])
                v3 = lambda ap: ap.rearrange("p (h f) -> p h f", h=8)
                P.op("dve", lambda e, k=k, qk=qk, cosb=cosb: e.tensor_tensor(out=v3(T1a[k]), in0=qk[:, :, 0, :], in1=cosb, op=ALU.mult),
                     reads=[tPS[0], tC], writes=[tt_["t1a"][k]])
                P.op("dve", lambda e, k=k, qk=qk, sinb=sinb: e.tensor_tensor(out=v3(T2a[k]), in0=qk[:, :, 1, :], in1=sinb, op=ALU.mult),
                     reads=[tPS[0], tC], writes=[tt_["t2a"][k]])
                P.op("dve", lambda e, k=k, qk=qk, cosb=cosb: e.tensor_tensor(out=v3(T1b[k]), in0=qk[:, :, 1, :], in1=cosb, op=ALU.mult),
                     reads=[tPS[0], tC], writes=[tt_["t1b"][k]])
                P.op("dve", lambda e, k=k, qk=qk, sinb=sinb: e.tensor_tensor(out=v3(T2b[k]), in0=qk[:, :, 0, :], in1=sinb, op=ALU.mult),
                     reads=[tPS[0], tC], writes=[tt_["t2b"][k]])
                P.op("dve", lambda e, k=k, qkr=qkr: e.tensor_tensor(out=qkr[:, :, 0, :], in0=v3(T1a[k]), in1=v3(T2a[k]), op=ALU.subtract),
                     reads=[tt_["t1a"][k], tt_["t2a"][k]], writes=[tt_["qkr"][k]])
                P.op("dve", lambda e, k=k, qkr=qkr: e.tensor_tensor(out=qkr[:, :, 1, :], in0=v3(T1b[k]), in1=v3(T2b[k]), op=ALU.add),
                     reads=[tt_["t1b"][k], tt_["t2b"][k]], writes=[tt_["qkr"][k]])
                proj_tok(1, t, 1)
                evac(VB[k], PS[1][:], [tPS[1]], [tt_["vb"][k]])
                proj_tok(2, t, 2)
                P.op("act", lambda e, k=k: e.activation(out=GS[k], in_=PS[2][:], func=AF.Silu), reads=[tPS[2]], writes=[tt_["gs"][k]])
                P.op("dve", lambda e, k=k: e.tensor_tensor(out=QKS[k][:, 0:256].rearrange("p (h f) -> p h f", h=4),
                                                           in0=QKR[k][:, 0:256].rearrange("p (h f) -> p h f", h=4),
                                                           in1=QD.unsqueeze(2).broadcast_to([128, 4, 64]), op=ALU.mult),
                     reads=[tt_["qkr"][k], tC], writes=[tt_["qks"][k]])
                P.op("dve", lambda e, k=k: e.tensor_tensor(out=KRD[k].rearrange("p (h f) -> p h f", h=4),
                                                           in0=QKR[k][:, 256:512].rearrange("p (h f) -> p h f", h=4),
                                                           in1=KD.unsqueeze(2).broadcast_to([128, 4, 64]), op=ALU.mult),
                     reads=[tt_["qkr"][k], tC], writes=[tt_["krd"][k]])
                P.op("pool", lambda e, k=k: e.tensor_scalar(out=QKS[k][:, 256:512], in0=QKR[k][:, 256:512], scalar1=0.125, scalar2=None,
                                                            op0=ALU.mult), reads=[tt_["qkr"][k]], writes=[tt_["qks"][k]])
                transposes(lambda b, k=k: QKS[k][:, b * 128:(b + 1) * 128], 4, [tt_["qks"][k]], 3,
                           QKT[k].rearrange("p (b c) -> p b c", b=4), [tt_["qrt"][k]])
                for h in range(4):
                    hp, hb = 64 * (h % 2), h // 2
                    P.op("dve", lambda e, h=h, hp=hp, hb=hb, k=k: e.tensor_copy(
                        out=KZ[k][hp:hp + 64, h, :], in_=QKT[k][hp:hp + 64, (2 + hb) * 128:(3 + hb) * 128]),
                        reads=[tt_["qrt"][k]], writes=[tKZ[k]])

            def ret_back(i):
                k = i % 2
                t = i
                qrt = QKT[k][:, 0:256].rearrange("p (b c) -> p b c", b=2)
                krt = QKT[k][:, 256:512].rearrange("p (b c) -> p b c", b=2)
                for h in range(4):
                    hp, hb = 64 * (h % 2), h // 2
                    P.op("pe", lambda e, h=h, hb=hb, k=k, qrt=qrt: e.matmul(
                        PS[4][:, h * 128:(h + 1) * 128], lhsT=KZ[k][:, h, :], rhs=qrt[:, hb, :], start=True, stop=True),
                        reads=[tKZ[k], tt_["qrt"][k]], writes=[tPS[4]])
                P.op("dve", lambda e, k=k: e.tensor_tensor(out=SCT[k].rearrange("p (h c) -> p h c", h=4),
                                                           in0=PS[4][:].rearrange("p (h c) -> p h c", h=4), in1=DM, op=ALU.mult),
                     reads=[tPS[4], tC], writes=[tt_["sct"][k]])
                sct = SCT[k].rearrange("p (h c) -> p h c", h=4)
                for h in range(4):
                    hp, hb = 64 * (h % 2), h // 2
                    P.op("pe", lambda e, h=h, sct=sct, k=k, i=i: e.matmul(
                        PS[5][:, h * 128:(h + 1) * 128], lhsT=sct[:, h, :], rhs=VB[k][:, h * 128:(h + 1) * 128], start=True, stop=(i == 0)),
                        reads=[tt_["sct"][k], tt_["vb"][k]], writes=[tPS[5]])
                    if i > 0:
                        P.op("pe", lambda e, h=h, hb=hb, qrt=qrt: e.matmul(
                            PS[5][:, h * 128:(h + 1) * 128], lhsT=qrt[:, hb, :], rhs=STB[:, h, :], start=False, stop=True),
                            reads=[tt_["qrt"][k], tSTB], writes=[tPS[5]])
                if i < NT - 1:
                    for hb in range(2):
                        P.op("pe", lambda e, hb=hb, k=k: e.matmul(
                            PS[6][:, hb * 256:(hb + 1) * 256], lhsT=KRD[k][:, hb * 128:(hb + 1) * 128], rhs=VB[k][:, hb * 256:(hb + 1) * 256],
                            start=True, stop=True), reads=[tt_["krd"][k], tt_["vb"][k]], writes=[tPS[6]])
                    for h in range(4):
                        hp, hb = 64 * (h % 2), h // 2
                        P.op("dve", lambda e, h=h, hp=hp, hb=hb: e.scalar_tensor_tensor(
                            out=ST32[hp:hp + 64, h, :], in0=ST32[hp:hp + 64, h, :], scalar=CD[h],
                            in1=PS[6][hp:hp + 64, hb * 256 + (h % 2) * 128:hb * 256 + (h % 2) * 128 + 128], op0=ALU.mult, op1=ALU.add),
                            reads=[tPS[6], tST], writes=[tST])
                    P.op("act", lambda e: e.copy(out=STB, in_=ST32), reads=[tST], writes=[tSTB])
                P.op("act", lambda e, k=k: e.copy(out=OSB[k], in_=PS[5][:]), reads=[tPS[5]], writes=[tt_["osb"][k]])
                stt = STT[k].rearrange("p (h s) -> p h s", h=4)
                mv = MV[k].rearrange("p (h s) -> p h s", h=4)
                tS = tt_["stt"][k]
                for h in range(4):
                    P.op("dve", lambda e, h=h, k=k, stt=stt: e.bn_stats(out=stt[:, h, :], in_=OSB[k][:, h * 128:(h + 1) * 128]),
                         reads=[tt_["osb"][k]], writes=[tS])
                for h in range(4):
                    P.op("dve", lambda e, h=h, stt=stt, mv=mv: e.bn_aggr(out=mv[:, h, 0:2], in_=stt[:, h, :]), reads=[tS], writes=[tS])
                P.op("act", lambda e, mv=mv: e.activation(out=mv[:, :, 2], in_=mv[:, :, 1], func=AF.Sqrt, bias=EPSB[:, 0:1]),
                     reads=[tS, tid], writes=[tS])
                P.op("dve", lambda e, mv=mv: e.reciprocal(out=mv[:, :, 2], in_=mv[:, :, 2]), reads=[tS], writes=[tS])
                P.op("dve", lambda e, mv=mv: e.scalar_tensor_tensor(out=mv[:, :, 3], in0=mv[:, :, 0], scalar=-1.0, in1=mv[:, :, 2],
                                                                    op0=ALU.mult, op1=ALU.mult), reads=[tS], writes=[tS])
                for h in range(4):
                    P.op("act", lambda e, h=h, k=k, mv=mv: e.activation(out=YN[k][:, h * 128:(h + 1) * 128], in_=OSB[k][:, h * 128:(h + 1) * 128],
                                                                        func=AF.Identity, scale=mv[:, h, 2:3], bias=mv[:, h, 3:4]),
                         reads=[tt_["osb"][k], tS], writes=[tt_["yn"][k]])
                P.op("dve", lambda e, k=k: e.tensor_tensor(out=GS[k].rearrange("p (h c) -> p h c", h=4),
                                                           in0=GS[k].rearrange("p (h c) -> p h c", h=4),
                                                           in1=RG.unsqueeze(1).broadcast_to([128, 4, 128]), op=ALU.mult),
                     reads=[tt_["gs"][k], tC], writes=[tt_["gs"][k]])
                P.op("dve", lambda e, k=k: e.tensor_tensor(out=YBt[k], in0=YN[k], in1=GS[k], op=ALU.mult),
                     reads=[tt_["yn"][k], tt_["gs"][k]], writes=[tt_["ybt"][k]])
                transposes(lambda kk, k=k: YBt[k][:, kk * 128:(kk + 1) * 128], 4, [tt_["ybt"][k]], 7,
                           YTb[:, :, t * 128:(t + 1) * 128], [tYTb[t]])


            for s_ in range(NT + 1):
                lists = []
                if s_ < NT:
                    P.begin_capture()
                    ret_front(s_)
                    lists.append(P.end_capture())
                if s_ >= 1:
                    P.begin_capture()
                    ret_back(s_ - 1)
                    lists.append(P.end_capture())
                P.replay(lists)

            P.barrier()
            ARENA.reset()
            QT = ARENA.take(4 * S * 2).rearrange("p (c f) -> p c f", c=4)
            KT = ARENA.take(4 * S * 2).rearrange("p (c f) -> p c f", c=4)
            V = ARENA.take(16 * 4 * 129 * 2).rearrange("p (t h e) -> p t h e", t=16, h=4)
            tQ = [T("q%d" % i) for i in range(4)]
            tK = [T("k%d" % i) for i in range(4)]
            tV = [T("v%d" % i) for i in range(NT)]
            load_w(0, d["w_in"][:, 0:512], 8)
            load_w(1, d["w_in"][:, 512:1024], 8)
            load_w(2, d["w_in"][:, 1024:1536], 8)
            proj_feat(0, QT, tQ, 4, [0, 1, 2, 3])
            proj_feat(1, KT, tK, 4, [0, 1, 2, 3])
            P.op("dve", lambda e: e.memset(V[:, :, :, 128:129], 1.0), writes=tV)
            for t in range(NT):
                b = 4 + t % 4
                proj_tok(2, t, b)
                evac(V[:, t, :, 0:128], PS[b][:].rearrange("p (h e) -> p h e", h=4), [tPS[b]], [tV[t]])
            P.barrier()
            RELB = LNP[:, 0:1024].rearrange("p (h j) -> p h j", h=4)
            YB4 = LNP[:, 1024:2048].bitcast(BF16).rearrange("p (a f) -> p a f", a=4)
            HF = HB[:, 1, :].bitcast(F32)
            LAM32 = HF[:, 0:256]
            DGB = HF[:, 256:384]
            LS = HF[:, 384:386]
            NL = HF[:, 386:387]
            CH = HF[:, 388:392]
            tL = T("lam")
            tR = T("relb")
            P.dma("sp", LAM32, d["lam"].broadcast_to([128, 256]), writes=[tL])
            P.dma("sp", DGB, d["dg"].broadcast_to([128, 128]), writes=[tL])
            P.dma("sp", LNP[:, 0:1024], d["relbT"], writes=[tR, tLNP])
            lamv = LAM32.rearrange("p (a b f) -> p a b f", a=2, b=2)
            P.op("dve", lambda e: e.tensor_tensor(out=lamv[:, :, 0, :], in0=lamv[:, :, 0, :], in1=lamv[:, :, 1, :], op=ALU.mult),
                 reads=[tL], writes=[tL])
            P.op("dve", lambda e: e.tensor_reduce(out=LS, in_=lamv[:, :, 0, :], axis=AX.X, op=ALU.add), reads=[tL], writes=[tL])
            P.op("act", lambda e: e.activation(out=LS, in_=LS, func=AF.Exp), reads=[tL], writes=[tL])
            P.op("dve", lambda e: e.tensor_scalar(out=NL, in0=LS[:, 1:2], scalar1=LS[:, 0:1], scalar2=-lam_init,
                                                  op0=ALU.subtract, op1=ALU.add), reads=[tL], writes=[tL])
            P.op("dve", lambda e: e.tensor_scalar(out=DGB, in0=DGB, scalar1=1.0 - lam_init, scalar2=None, op0=ALU.mult),
                 reads=[tL], writes=[tL])
            P.op("dve", lambda e: e.tensor_copy(out=CH, in_=RELB[:, :, 255]), reads=[tR], writes=[tL])
            for h in range(4):
                P.op("dve", lambda e, h=h: e.tensor_scalar(out=RELB[:, h, :], in0=RELB[:, h, :], scalar1=CH[:, h:h + 1], scalar2=None,
                                                           op0=ALU.subtract), reads=[tR, tL], writes=[tR])
            P.op("pool", lambda e: e.affine_select(out=RELB, in_=RELB, pattern=[[0, 4], [1, 256]], compare_op=ALU.is_ge, fill=-30000.0,
                                                   base=0, channel_multiplier=-1), reads=[tR], writes=[tR])
            EPF = HB[:, 0, :].bitcast(F32)
            tEP = [T("ep0"), T("ep1")]
            tYB4 = [T("yb4%d" % i) for i in range(4)]
            epk = [0]
            btk = [0, 0]

            def bias_fn(u, g, j):
                return 0.0, []

            def near_fn(u, g, j, ilo, nb, sap, st_, pap, pt_, stream):
                h = u // 2
                if ilo == j:
                    nn, boff = min(2, nb), 0
                elif ilo == j + 1:
                    nn, boff = 1, 128
                else:
                    return 0
                k = 2 * stream + btk[stream] % 2
                btk[stream] += 1
                P.op("dve", lambda e: e.scalar_tensor_tensor(out=BT[:, k, 0:nn * 128], in0=sap[:, 0:nn * 128], scalar=0.125,
                                                             in1=RELB[:, h, boff:boff + nn * 128], op0=ALU.mult, op1=ALU.add),
                     reads=[st_, tR], writes=[tBT[k]])
                P.op("act", lambda e: e.activation(out=pap[:, 0:nn * 128], in_=BT[:, k, 0:nn * 128], func=AF.Exp),
                     reads=[tBT[k]], writes=[pt_])
                return nn

            def acc_map(u, i):
                c = u % 2
                if i < 3:
                    return 4 + 2 * c, i * 129
                return 5 + 2 * c, 0

            def epilogue(u, g, acc, acc_t):
                if u % 2 == 0:
                    return
                h = u // 2
                for i in range(4):
                    k = epk[0] % 2
                    epk[0] += 1
                    b1, o1 = acc_map(u - 1, i)
                    b2, o2 = acc_map(u, i)
                    O1 = PS[b1][:, o1:o1 + 129]
                    O2 = PS[b2][:, o2:o2 + 129]
                    TA = EPF[:, k * 256:k * 256 + 128]
                    YA = EPF[:, k * 256 + 128:k * 256 + 256]
                    RS = SM[:, 400 + k * 8:400 + k * 8 + 8]
                    te = tEP[k]
                    P.op("dve", lambda e, O1=O1, RS=RS: e.reciprocal(out=RS[:, 0:1], in_=O1[:, 128:129]), reads=[tPS[b1]], writes=[te])
                    P.op("dve", lambda e, O2=O2, RS=RS: e.reciprocal(out=RS[:, 1:2], in_=O2[:, 128:129]), reads=[tPS[b2]], writes=[te])
                    P.op("dve", lambda e, RS=RS: e.tensor_tensor(out=RS[:, 2:3], in0=RS[:, 1:2], in1=NL, op=ALU.mult), reads=[te, tL], writes=[te])
                    P.op("dve", lambda e, O2=O2, RS=RS, TA=TA: e.tensor_scalar(out=TA, in0=O2[:, 0:128], scalar1=RS[:, 2:3], scalar2=None,
                                                                               op0=ALU.mult), reads=[tPS[b2], te], writes=[te])
                    P.op("dve", lambda e, O1=O1, RS=RS, TA=TA, YA=YA: e.scalar_tensor_tensor(
                        out=YA, in0=O1[:, 0:128], scalar=RS[:, 0:1], in1=TA, op0=ALU.mult, op1=ALU.add), reads=[tPS[b1], te], writes=[te])
                    P.op("act", lambda e, RS=RS, TA=TA, YA=YA: e.activation(out=TA, in_=YA, func=AF.Square, accum_out=RS[:, 3:4]),
                         reads=[te], writes=[te])
                    P.op("act", lambda e, RS=RS: e.activation(out=RS[:, 4:5], in_=RS[:, 3:4], func=AF.Sqrt, scale=1.0 / 128.0, bias=EPSB[:, 0:1]),
                         reads=[te, tid], writes=[te])
                    P.op("dve", lambda e, RS=RS: e.reciprocal(out=RS[:, 4:5], in_=RS[:, 4:5]), reads=[te], writes=[te])
                    P.op("dve", lambda e, RS=RS, YA=YA, i=i, h=h: e.scalar_tensor_tensor(
                        out=YB4[:, i, h * 128:(h + 1) * 128], in0=YA, scalar=RS[:, 4:5], in1=DGB, op0=ALU.mult, op1=ALU.mult),
                        reads=[te, tL], writes=[tYB4[i]])

            def after_group(g):
                for ii in range(4):
                    t = 4 * g + ii
                    transposes(lambda kk, ii=ii: YB4[:, ii, kk * 128:(kk + 1) * 128], 4, [tYB4[ii]], ii % 2,
                               XT[:, 0:4, t * 128:(t + 1) * 128], [tXT[t]])

            attention(8, lambda u: 64 * (u % 2), lambda u: u // 2, lambda u: u // 2, QT, KT, tQ, tK, V, tV, 129, 4,
                      bias_fn, near_fn, epilogue, after_group, acc_map, False)

            def ychunk(c):
                if c < 4:
                    return XT[:, c, :], tXT
                return YTb[:, c - 4, :], tYTb
            out_proj_ln1(l, ychunk)

        for li, l in enumerate(layers):
            if l == 1:
                layer_odd(l)
            else:
                layer_even(l)
            if STOP == "M":
                P.barrier()
                for t in range(NT):
                    P.dma("sp", y_d[t * 128:(t + 1) * 128, :], X[:, t, :], reads=[tX[t]])
                break
            ffn_block(l, li == len(layers) - 1)
            continue
            if STOP:
                P.barrier()
                for t in range(NT):
                    P.dma("sp", y_d[t * 128:(t + 1) * 128, :], X[:, t, :], reads=[tX[t]])
                break
            ffn_block(l, li == len(layers) - 1)

        P.emit()
    return nc


_NC_CACHE = {}


def _get_nc(layers):
    key = tuple(layers)
    if key not in _NC_CACHE:
        _NC_CACHE[key] = build(list(layers))
    return _NC_CACHE[key]


def _layer_inputs(l, b, inp):
    f = lambda a: np.ascontiguousarray(a, dtype=np.float32)
    d = {}
    j = l // 2
    d["p%d" % l] = f(inp["p"][l, b])
    d["lnp%d" % l] = f(np.stack([inp["ln_mix_g"][l], inp["ln_mix_b"][l], inp["ln_ffn_g"][l], inp["ln_ffn_b"][l]]))
    d["wg%d" % l] = f(inp["moe_w_gate"][l])
    d["wu%d" % l] = f(inp["moe_w_up"][l])
    d["wd%d" % l] = f(inp["moe_w_down"][l])
    d["pproj%d" % l] = f(inp["ple_proj"][l])
    d["pgate%d" % l] = f(inp["ple_gate"][l])
    if l % 2 == 0:
        d["w_in%d" % l] = f(inp["even_w_in"][j])
        d["w_out%d" % l] = f(inp["even_w_out"][j])
        pp = np.arange(128)[:, None]
        jj = np.arange(256)[None, :]
        bucket = _t5_bucket_np(np.maximum(jj - pp, 0))
        rb = np.asarray(inp["rel_bias"], np.float32)
        g = rb[bucket]
        d["relbT"] = f(np.transpose(g, (0, 2, 1)).reshape(128, 1024))
        d["lam"] = f(np.asarray(inp["even_lambda"][j]).reshape(1, 256))
        d["dg"] = f(np.asarray(inp["even_diff_norm"][j]).reshape(1, 128))
        d["rg"] = f(np.asarray(inp["even_ret_norm"][j]).reshape(1, 128))
    else:
        d["w_in%d" % l] = f(inp["odd_w_in"][j])
        d["w_out%d" % l] = f(inp["odd_w_out"][j])
        d["bf"] = f(np.asarray(inp["odd_b_forget"][j]).reshape(1, 16))
    return d


def run_layers(layers, xin, inp):
    nc = _get_nc(layers)
    consts, _ = _static_consts()
    in_maps = []
    for b in range(8):
        m = {"x": np.ascontiguousarray(xin[b], dtype=np.float32), "consts": consts,
             "router_w": np.ascontiguousarray(inp["router_w"], dtype=np.float32)}
        for l in layers:
            m.update(_layer_inputs(l, b, inp))
        in_maps.append(m)
    res = run_bass_kernel_spmd(nc, in_maps, core_ids=list(range(8)))
    return np.stack([np.asarray(r["y"], dtype=np.float32) for r in res.results], axis=0)


def kernel(**inputs):
    inp = {k: np.asarray(v) for k, v in inputs.items()}
    x = np.asarray(inp["x"], dtype=np.float32)
    x = run_layers([0, 1], x, inp)
    return x.astype(np.float32)
```
